# Optimizing a Trainium2 kernel written in Bass

```python
import math
import jax, jax.numpy as jnp
from jax import lax
import numpy as np


D_MODEL = 1024
BATCH = 2
SEQ = 16384
DEPTH = 1

MIX_WIDTH = D_MODEL
HG_WIDTH = MIX_WIDTH // 2
HG_HEADS = 4
HG_DK = HG_WIDTH // HG_HEADS
HG_CHUNK = 64
S5_WIDTH = MIX_WIDTH - HG_WIDTH
S5_GROUP = 16
S5_GROUPS = S5_WIDTH // S5_GROUP
S5_STATE = 64
IN_WIDTH = 4 * HG_WIDTH + S5_WIDTH
PEER_HEADS = 8
PEER_NKEYS = 128
PEER_EXPERTS = PEER_NKEYS * PEER_NKEYS
PEER_TOPK = 16
PEER_QDIM = 256
PEER_HALF = PEER_QDIM // 2
PEER_TOKEN_BLOCK = 128
EPS = 1e-6

kernel_name = 'hymba_hgrn2_s5_peer_adaln'


def _rmsnorm(x, w):
    xf = x.astype(jnp.float32)
    y = xf * lax.rsqrt(jnp.mean(xf * xf, axis=-1, keepdims=True) + EPS)
    return (y * w.astype(jnp.float32)).astype(x.dtype)


def _hgrn2(q, f_logit, i, g, lb, gn_w):
    bsz, seq, _ = q.shape
    f32 = jnp.float32
    f = lb + (1.0 - lb) * jax.nn.sigmoid(f_logit.astype(f32))
    log_f = jnp.log(f)
    k = 1.0 - f
    qs = jax.nn.silu(q.astype(f32)) * (HG_DK ** -0.5)
    v = i.astype(f32)
    n_chunks = seq // HG_CHUNK

    def to_chunks(t):
        return t.reshape(bsz, n_chunks, HG_CHUNK, HG_HEADS, HG_DK).transpose(1, 0, 3, 2, 4)

    causal = jnp.tril(jnp.ones((HG_CHUNK, HG_CHUNK), dtype=bool))[:, :, None]

    def step(state, inp):
        qc, kc, vc, lc = inp
        bcum = jnp.cumsum(lc, axis=2)
        o_inter = jnp.einsum('bhtk,bhkv->bhtv', qc * jnp.exp(bcum), state)
        diff = bcum[:, :, :, None, :] - bcum[:, :, None, :, :]
        decay = jnp.exp(jnp.where(causal, diff, -jnp.inf))
        scores = jnp.einsum('bhtk,bhsk,bhtsk->bhts', qc, kc, decay)
        o_intra = jnp.einsum('bhts,bhsv->bhtv', scores, vc)
        b_last = bcum[:, :, -1:, :]
        new_state = (jnp.exp(b_last[:, :, 0, :])[..., None] * state
                     + jnp.einsum('bhsk,bhsv->bhkv', kc * jnp.exp(b_last - bcum), vc))
        return new_state, o_inter + o_intra

    s0 = jnp.zeros((bsz, HG_HEADS, HG_DK, HG_DK), f32)
    _, o = lax.scan(step, s0, (to_chunks(qs), to_chunks(k), to_chunks(v), to_chunks(log_f)))
    o = o.transpose(1, 0, 3, 2, 4).reshape(bsz, seq, HG_HEADS, HG_DK)
    gate = jax.nn.silu(g.astype(f32)).reshape(bsz, seq, HG_HEADS, HG_DK)
    o = _rmsnorm(o, gn_w) * gate
    return o.reshape(bsz, seq, HG_WIDTH)


def _s5(u, a_re, a_im, log_dt, b_re, b_im, c_re, c_im, d_skip, w_glu, b_glu):
    bsz, seq, _ = u.shape
    f32 = jnp.float32
    uf = u.astype(f32).reshape(bsz, seq, S5_GROUPS, S5_GROUP)
    ar = a_re.astype(f32)
    ai = a_im.astype(f32)
    dt = jnp.exp(log_dt.astype(f32))[:, None]
    mag = jnp.exp(ar * dt)
    abar_re = mag * jnp.cos(ai * dt)
    abar_im = mag * jnp.sin(ai * dt)
    den = ar * ar + ai * ai
    nr = abar_re - 1.0
    ni = abar_im
    coef_re = ((nr * ar + ni * ai) / den)[..., None]
    coef_im = ((ni * ar - nr * ai) / den)[..., None]
    br = b_re.astype(f32)
    bi = b_im.astype(f32)
    bb_re = coef_re * br - coef_im * bi
    bb_im = coef_re * bi + coef_im * br
    bu_re = jnp.einsum('bsgh,gph->bsgp', uf, bb_re)
    bu_im = jnp.einsum('bsgh,gph->bsgp', uf, bb_im)
    a_re_t = jnp.broadcast_to(abar_re, bu_re.shape)
    a_im_t = jnp.broadcast_to(abar_im, bu_im.shape)

    def combine(e1, e2):
        a1r, a1i, x1r, x1i = e1
        a2r, a2i, x2r, x2i = e2
        return (a2r * a1r - a2i * a1i,
                a2r * a1i + a2i * a1r,
                a2r * x1r - a2i * x1i + x2r,
                a2r * x1i + a2i * x1r + x2i)

    _, _, xr, xi = lax.associative_scan(combine, (a_re_t, a_im_t, bu_re, bu_im), axis=1)
    y = (jnp.einsum('bsgp,ghp->bsgh', xr, c_re.astype(f32))
         - jnp.einsum('bsgp,ghp->bsgh', xi, c_im.astype(f32)))
    y = y.reshape(bsz, seq, S5_WIDTH) + d_skip.astype(f32) * uf.reshape(bsz, seq, S5_WIDTH)
    y = jax.nn.gelu(y, approximate=False)
    y = y * jax.nn.sigmoid(y @ w_glu.astype(f32) + b_glu.astype(f32))
    return y.astype(u.dtype)


def _peer(h, w_q, keys1, keys2, u_tab, v_tab):
    bsz, seq, d = h.shape
    tb = PEER_TOKEN_BLOCK
    blocks = h.reshape(-1, tb, d)
    kk = PEER_TOPK * PEER_TOPK

    def block(hb):
        qh = (hb @ w_q).reshape(tb, PEER_HEADS, 2, PEER_HALF)
        s1 = jnp.einsum('thd,nd->thn', qh[:, :, 0], keys1)
        s2 = jnp.einsum('thd,nd->thn', qh[:, :, 1], keys2)
        v1, i1 = lax.top_k(s1, PEER_TOPK)
        v2, i2 = lax.top_k(s2, PEER_TOPK)
        cand = (v1[..., :, None] + v2[..., None, :]).reshape(tb, PEER_HEADS, kk)
        cidx = (i1[..., :, None] * PEER_NKEYS + i2[..., None, :]).reshape(tb, PEER_HEADS, kk)
        best, pos = lax.top_k(cand, PEER_TOPK)
        eidx = jnp.take_along_axis(cidx, pos, axis=-1)
        gates = jax.nn.softmax(best.astype(jnp.float32), axis=-1).astype(hb.dtype)
        act = jax.nn.gelu(jnp.einsum('td,thkd->thk', hb, u_tab[eidx]), approximate=False)
        return jnp.einsum('thk,thkd->td', gates * act, v_tab[eidx])

    return lax.map(block, blocks).reshape(bsz, seq, d)


def setup_inputs(seed: int = 0) -> dict:
    key = jax.random.key(seed)
    ks = jax.random.split(key, 26)
    f32 = jnp.float32

    def nrm(k, shape, std):
        return std * jax.random.normal(k, shape, f32)

    L = DEPTH
    D = D_MODEL
    G = S5_GROUPS
    P = S5_STATE
    return {
        'x': nrm(ks[0], (BATCH, SEQ, D), 1.0),
        'c': nrm(ks[1], (BATCH, D), 1.0),
        'ada_w': nrm(ks[2], (L, D, 6 * D), 0.5 * D ** -0.5),
        'ada_b': nrm(ks[3], (L, 6 * D), 0.01),
        'norm_mix_w': 1.0 + nrm(ks[4], (L, D), 0.02),
        'norm_ffn_w': 1.0 + nrm(ks[5], (L, D), 0.02),
        'w_in': nrm(ks[6], (L, D, IN_WIDTH), D ** -0.5),
        'w_out': nrm(ks[7], (L, MIX_WIDTH, D), MIX_WIDTH ** -0.5),
        'hg_lower_bounds': nrm(ks[8], (L + 1, HG_WIDTH), 0.1),
        'hg_gnorm_w': 1.0 + nrm(ks[9], (L, HG_DK), 0.02),
        's5_a_re': -0.5 + nrm(ks[10], (L, G, P), 0.01),
        's5_a_im': math.pi * jnp.arange(P, dtype=f32) + nrm(ks[11], (L, G, P), 0.01),
        's5_log_dt': jax.random.uniform(ks[12], (L, G), f32, math.log(1e-3), math.log(1e-1)),
        's5_b_re': nrm(ks[13], (L, G, P, S5_GROUP), (2 * S5_GROUP) ** -0.5),
        's5_b_im': nrm(ks[14], (L, G, P, S5_GROUP), (2 * S5_GROUP) ** -0.5),
        's5_c_re': nrm(ks[15], (L, G, S5_GROUP, P), 0.25),
        's5_c_im': nrm(ks[16], (L, G, S5_GROUP, P), 0.25),
        's5_d': nrm(ks[17], (L, S5_WIDTH), 0.5),
        's5_glu_w': nrm(ks[18], (L, S5_WIDTH, S5_WIDTH), S5_WIDTH ** -0.5),
        's5_glu_b': nrm(ks[19], (L, S5_WIDTH), 0.01),
        'peer_wq': nrm(ks[20], (L, D, PEER_HEADS * PEER_QDIM), D ** -0.5),
        'peer_keys1': nrm(ks[21], (L, PEER_NKEYS, PEER_HALF), PEER_HALF ** -0.5),
        'peer_keys2': nrm(ks[22], (L, PEER_NKEYS, PEER_HALF), PEER_HALF ** -0.5),
        'peer_u': nrm(ks[23], (L, PEER_EXPERTS, D), D ** -0.5),
        'peer_v': nrm(ks[24], (L, PEER_EXPERTS, D), PEER_HEADS ** -0.5),
        'final_norm_w': 1.0 + nrm(ks[25], (D,), 0.02),
    }


def reference(x, c, ada_w, ada_b, norm_mix_w, norm_ffn_w, w_in, w_out, hg_lower_bounds,
              hg_gnorm_w, s5_a_re, s5_a_im, s5_log_dt, s5_b_re, s5_b_im, s5_c_re, s5_c_im,
              s5_d, s5_glu_w, s5_glu_b, peer_wq, peer_keys1, peer_keys2, peer_u, peer_v,
              final_norm_w):
    cond = jax.nn.silu(c)
    lb_all = jnp.cumsum(jax.nn.softmax(hg_lower_bounds.astype(jnp.float32), axis=0), axis=0)
    for l in range(DEPTH):
        mod = (cond @ ada_w[l] + ada_b[l])[:, None, :]
        sh1, sc1, gt1, sh2, sc2, gt2 = jnp.split(mod, 6, axis=-1)
        h = _rmsnorm(x, norm_mix_w[l]) * (1.0 + sc1) + sh1
        proj = h @ w_in[l]
        q, f_logit, i, g, u = jnp.split(
            proj, [HG_WIDTH, 2 * HG_WIDTH, 3 * HG_WIDTH, 4 * HG_WIDTH], axis=-1)
        o_hg = _hgrn2(q, f_logit, i, g, lb_all[l], hg_gnorm_w[l]).astype(x.dtype)
        o_s5 = _s5(u, s5_a_re[l], s5_a_im[l], s5_log_dt[l], s5_b_re[l], s5_b_im[l],
                   s5_c_re[l], s5_c_im[l], s5_d[l], s5_glu_w[l], s5_glu_b[l])
        mixed = jnp.concatenate([o_hg, o_s5], axis=-1) @ w_out[l]
        x = x + gt1 * mixed
        h2 = _rmsnorm(x, norm_ffn_w[l]) * (1.0 + sc2) + sh2
        x = x + gt2 * _peer(h2, peer_wq[l], peer_keys1[l], peer_keys2[l], peer_u[l], peer_v[l])
    return _rmsnorm(x, final_norm_w)
```

```python
import math
import os
import numpy as np
from contextlib import ExitStack
import concourse.bass as bass
import concourse.mybir as mybir
from concourse.bass_utils import run_bass_kernel_spmd

F32 = mybir.dt.float32
BF16 = mybir.dt.bfloat16
I32 = mybir.dt.int32
U32 = mybir.dt.uint32
AF = mybir.ActivationFunctionType
ALU = mybir.AluOpType
AX = mybir.AxisListType

D = 1024
KC = 8
DK = 128
TC = 256
EPS = 1e-6
TWO_PI = 2.0 * math.pi


def _isap(v):
    return type(v).__name__ == 'AP'


class Prog:
    def __init__(self, nc):
        self.nc = nc
        self.es = ExitStack()
        self.eng = {'pe': nc.tensor, 'dve': nc.vector, 'act': nc.scalar, 'pool': nc.gpsimd, 'sp': nc.sync}
        self.sem = {k: self.es.enter_context(nc.semaphore("s_" + k)) for k in self.eng}
        self.cnt = {k: 0 for k in self.eng}
        self.waited = {k: {} for k in self.eng}
        self.writers = {}
        self.readers = {}
        self.dmasem = {}
        self.allmax = {}
        self.ninst = 0

    def _wait(self, e, toks):
        for (name, sem, val) in toks:
            if name == 'pe' and e == 'pe':
                continue
            if self.waited[e].get(name, 0) < val:
                self.eng[e].wait_ge(sem, val)
                self.waited[e][name] = val

    def _deps(self, e, rk, wk):
        toks = []
        for b in rk:
            toks += list(self.writers.get(b, {}).values())
        for b in wk:
            toks += list(self.writers.get(b, {}).values())
            toks += list(self.readers.get(b, {}).values())
        self._wait(e, toks)

    def _record(self, tok, rk, wk):
        for b in rk:
            self.readers.setdefault(b, {})[tok[0]] = tok
        for b in wk:
            self.writers.setdefault(b, {})[tok[0]] = tok
        self.allmax[tok[0]] = tok

    def I(self, e, meth, **kw):
        outs, ins = [], []
        for k, v in kw.items():
            if _isap(v):
                (outs if (k in ('out', 'accum_out', 'ap') or v.space == 'PSUM') else ins).append(v.name)
        self._deps(e, ins, outs)
        ins_obj = getattr(self.eng[e], meth)(**kw)
        self.cnt[e] += 1
        self.ninst += 1
        ins_obj.then_inc(self.sem[e], 1)
        self._record((e, self.sem[e], self.cnt[e]), ins, outs)

    def _dsem(self, key):
        if key not in self.dmasem:
            self.dmasem[key] = [self.es.enter_context(self.nc.semaphore("d_" + key)), 0]
        s = self.dmasem[key]
        s[1] += 16
        return s

    def dma(self, e, out, in_):
        rk, wk = [in_.name], [out.name]
        self._deps(e, rk, wk)
        key = out.name if out.space != 'DRAM' else in_.name
        s = self._dsem(key)
        self.eng[e].dma_start(out=out, in_=in_).then_inc(s[0], 16)
        self.ninst += 1
        self._record(('d_' + key, s[0], s[1]), rk, wk)

    def gather(self, out, table, idx):
        rk, wk = [table.name, idx.name], [out.name]
        self._deps('pool', rk, wk)
        s = self._dsem(out.name)
        self.nc.gpsimd.indirect_dma_start(
            out=out, out_offset=None, in_=table,
            in_offset=bass.IndirectOffsetOnAxis(ap=idx, axis=0)).then_inc(s[0], 16)
        self.ninst += 1
        self._record(('d_' + out.name, s[0], s[1]), rk, wk)

    def barrier(self):
        toks = list(self.allmax.values())
        for e in self.eng:
            self._wait(e, toks)

    def finish(self):
        self._wait('sp', list(self.allmax.values()))


def build(SEG, dbg=False, phases="ABC", lim=99):
    NPOS = 4 * SEG
    NBLK = NPOS // 512
    BPS = SEG // 512
    NT = SEG // 128
    nc = bass.Bass("TRN2", target_bir_lowering=False)

    def din(name, shape, dt=F32):
        return nc.dram_tensor(name, list(shape), dt, kind="ExternalInput").ap()

    xin = din("xin", [NPOS, D])
    maskc_d = din("maskc", [128, 4])
    cT_d = din("cT", [128, 8])
    adaw_d = din("ada_w", [D, 6 * D])
    adabc_d = din("ada_bc", [128, 48])
    adabr_d = din("ada_br", [1, 6 * D])
    nmw_d = din("nmw_c", [128, 8])
    nfw_d = din("nfw_c", [128, 8])
    nfwr_d = din("nfw_r", [1, D])
    fnw_d = din("fnw_b", [128, D])
    win_d = din("w_in", [D, 2560])
    wout_d = din("w_out", [D, D])
    wq_d = din("wq", [D, 2048])
    glu_d = din("glu_w", [512, 512])
    hlb_d = din("hlb_b", [128, 2, 512])
    gnw_d = din("gnw_b", [128, 128])
    s5sc_d = din("s5sc", [128, 16, 3])
    bpad_d = din("bpad", [16, 128, 256])
    cpad_d = din("cpad", [16, 128, 256])
    s5d_d = din("s5d_c", [128, 4])
    glub_d = din("glub_c", [128, 4])
    keysT_d = din("keysT", [128, 256])
    if "C" in phases:
        uvtab_d = din("uv_tab", [16384, 2 * D])
        uvb = nc.dram_tensor("uvb", [16384, 2 * D], BF16, kind="Internal").ap()
    ident_d = din("ident", [128, 128])
    tri2_d = din("tri2", [128, 128])
    suf2_d = din("suf2", [128, 128])
    chunkind_d = din("chunkind", [128, 2])
    iota16_d = din("iota16", [128, 16])
    out_d = nc.dram_tensor("out", [SEG, D], F32, kind="ExternalOutput").ap()
    x1s = nc.dram_tensor("x1s", [SEG, D], F32, kind="ExternalOutput" if dbg else "Internal").ap()

    P = Prog(nc)
    es = P.es
    _n = [0]

    def sb(es_, shape, dt=F32, name=None):
        _n[0] += 1
        return es_.enter_context(nc.sbuf_tensor("sb_" + (name or ("t%d" % _n[0])), list(shape), dt))

    def ps(es_, shape, dt=F32, name=None):
        _n[0] += 1
        return es_.enter_context(nc.psum_tensor("ps_" + (name or ("p%d" % _n[0])), list(shape), dt))

    ident_f = sb(es, [128, 128], name="ident_f")
    ident_b = sb(es, [128, 128], BF16, name="ident_b")
    tri2 = sb(es, [128, 128], name="tri2")
    suf2 = sb(es, [128, 128], name="suf2")
    chunkind = sb(es, [128, 2], name="chunkind")
    ones_row = sb(es, [1, 128], name="ones_row")
    gt1_b = sb(es, [128, D], name="gt1_b")
    bcs = nc.dram_tensor("bcs", [3, 128, D], F32, kind="Internal").ap()
    scale1m = sb(es, [128, 4, 8], name="scale1m")
    bias1m = sb(es, [128, 4, 8], name="bias1m")
    scale2c = sb(es, [128, 8], name="scale2c")
    sh2c = sb(es, [128, 8], name="sh2c")

    for (t, d_) in ((ident_f, ident_d), (tri2, tri2_d), (suf2, suf2_d), (chunkind, chunkind_d)):
        P.dma('sp', out=t[:], in_=d_[:, :])
    P.I('dve', 'tensor_copy', out=ident_b[:], in_=ident_f[:])
    P.I('dve', 'memset', ap=ones_row[:], constant=1.0)

    if "C" in phases:
        with ExitStack() as s0:
            cf = [sb(s0, [128, 4, 2 * D], name="castf%d" % i) for i in range(2)]
            cb16 = [sb(s0, [128, 4, 2 * D], BF16, name="castb%d" % i) for i in range(2)]
            uv_v = uvtab_d.rearrange("(c p i) d -> c p i d", p=128, i=4)
            uvb_v = uvb.rearrange("(c p i) d -> c p i d", p=128, i=4)
            for c in range(32):
                P.dma('sp' if c % 2 == 0 else 'act', out=cf[c % 2][:], in_=uv_v[c])
                e3 = ('dve', 'pool', 'act')[c % 3]
                if e3 == 'act':
                    P.I('act', 'activation', out=cb16[c % 2][:], in_=cf[c % 2][:], func=AF.Identity)
                else:
                    P.I(e3, 'tensor_copy', out=cb16[c % 2][:], in_=cf[c % 2][:])
                P.dma('sp' if c % 2 == 1 else 'act', out=uvb_v[c], in_=cb16[c % 2][:])
        P.barrier()

    with ExitStack() as sa:
        cT = sb(sa, [128, 8], name="cT")
        cond = sb(sa, [128, 8], name="cond")
        adaw = [sb(sa, [128, 8, 512], name="adaw%d" % i) for i in range(2)]
        adabc = sb(sa, [128, 48], name="adabc")
        adabr = sb(sa, [1, 6 * D], name="adabr")
        rowbuf = sb(sa, [1, 4 * D], name="rowbuf")
        nmw = sb(sa, [128, 8], name="nmw")
        nfw = sb(sa, [128, 8], name="nfw")
        nfwr = sb(sa, [1, D], name="nfwr")
        maskc = sb(sa, [128, 4], name="maskc")
        modc = sb(sa, [128, 48], name="modc")
        tmp8 = sb(sa, [128, 8], name="tmp8")
        scale1 = sb(sa, [128, 8], name="scale1")
        rtmp = sb(sa, [1, D], name="rtmp")
        modc_ps = ps(sa, [128, 512], name="modc_ps")
        row_ps = ps(sa, [128, 512], name="row_ps")
        bc_ps = ps(sa, [128, 512], name="bc_ps")

        for (t, d_) in ((cT, cT_d), (adabc, adabc_d), (adabr, adabr_d), (nmw, nmw_d), (nfw, nfw_d),
                        (nfwr, nfwr_d), (maskc, maskc_d)):
            P.dma('sp', out=t[:], in_=d_[:, :])
        P.I('act', 'activation', out=cond[:], in_=cT[:], func=AF.Silu)
        adaw_v = adaw_d.rearrange("(k p) c -> p k c", p=128)
        for jc in range(12):
            aw = adaw[jc % 2]
            P.dma('sp' if jc % 2 == 0 else 'act', out=aw[:], in_=adaw_v[:, :, jc * 512:(jc + 1) * 512])
            fam = jc // 2
            if True:
                for cb in range(4):
                    j = jc * 4 + cb
                    for k in range(KC):
                        P.I('pe', 'matmul', out=modc_ps[:, j:j + 1], lhsT=aw[:, k, cb * 128:(cb + 1) * 128],
                            rhs=cond[:, k:k + 1], start=(k == 0), stop=(k == KC - 1))
            if fam in (2, 3, 4, 5):
                for k in range(KC):
                    P.I('pe', 'matmul', out=row_ps[0:1, 0:512], lhsT=cond[:, k:k + 1], rhs=aw[:, k, :],
                        start=(k == 0), stop=(k == KC - 1))
                off = (fam - 2) * D + (jc % 2) * 512
                P.I('dve', 'tensor_tensor', out=rowbuf[0:1, off:off + 512], in0=row_ps[0:1, 0:512],
                    in1=adabr[0:1, jc * 512:(jc + 1) * 512], op=ALU.add)
        P.I('dve', 'tensor_tensor', out=modc[:], in0=modc_ps[:, 0:48], in1=adabc[:], op=ALU.add)
        P.I('dve', 'tensor_scalar', out=tmp8[:], in0=modc[:, 8:16], scalar1=1.0, scalar2=None, op0=ALU.add)
        P.I('dve', 'tensor_tensor', out=scale1[:], in0=tmp8[:], in1=nmw[:], op=ALU.mult)
        P.I('dve', 'tensor_scalar', out=tmp8[:], in0=modc[:, 32:40], scalar1=1.0, scalar2=None, op0=ALU.add)
        P.I('dve', 'tensor_tensor', out=scale2c[:], in0=tmp8[:], in1=nfw[:], op=ALU.mult)
        P.I('dve', 'tensor_copy', out=sh2c[:], in_=modc[:, 24:32])
        for s in range(4):
            P.I('dve', 'tensor_scalar', out=scale1m[:, s, :], in0=scale1[:], scalar1=maskc[:, s:s + 1],
                scalar2=None, op0=ALU.mult)
            P.I('dve', 'tensor_scalar', out=bias1m[:, s, :], in0=modc[:, 0:8], scalar1=maskc[:, s:s + 1],
                scalar2=None, op0=ALU.mult)
        P.I('dve', 'tensor_scalar', out=rtmp[:], in0=rowbuf[0:1, 2 * D:3 * D], scalar1=1.0, scalar2=None, op0=ALU.add)
        P.I('dve', 'tensor_tensor', out=rowbuf[0:1, 2 * D:3 * D], in0=rtmp[:], in1=nfwr[:], op=ALU.mult)
        bct = sb(sa, [128, D], name="bct")
        for ri in range(4):
            dst = gt1_b if ri == 0 else bct
            for hf in range(2):
                o = ri * D + hf * 512
                P.I('pe', 'matmul', out=bc_ps[:, 0:512], lhsT=ones_row[0:1, 0:128], rhs=rowbuf[0:1, o:o + 512],
                    start=True, stop=True)
                P.I('act', 'activation', out=dst[:, hf * 512:(hf + 1) * 512], in_=bc_ps[:, 0:512], func=AF.Identity)
            if ri > 0:
                P.dma('sp', out=bcs[ri - 1, :, :], in_=bct[:])
    P.barrier()

    if "B" in phases and lim >= 2:
      with ExitStack() as sB:
        win_bf = sb(sB, [128, KC, 2560], BF16, name="win_bf")
        wout_bf = sb(sB, [128, KC, D], BF16, name="wout_bf")
        glu_bf = sb(sB, [128, 4, 512], BF16, name="glu_bf")
        lb_b = sb(sB, [128, 512], name="lb_b")
        oml_b = sb(sB, [128, 512], name="oml_b")
        gnw_b = sb(sB, [128, 128], name="gnw_b")
        s5d_c = sb(sB, [128, 4], name="s5d_c")
        glub_c = sb(sB, [128, 4], name="glub_c")
        cosT = sb(sB, [128, 16, TC], name="cosT")
        sinT = sb(sB, [128, 16, TC], name="sinT")
        mag_p = sb(sB, [128, 16], name="mag_p")
        cs_p = sb(sB, [128, 16], name="cs_p")
        sn_p = sb(sB, [128, 16], name="sn_p")
        A256r = sb(sB, [128, 16], name="A256r")
        A256i = sb(sB, [128, 16], name="A256i")
        BbT = sb(sB, [128, 16, 256], BF16, name="BbT")
        Cb = sb(sB, [128, 16, 256], BF16, name="Cb")
        car_re = sb(sB, [128, 16], name="car_re")
        car_im = sb(sB, [128, 16], name="car_im")
        Sst = [sb(sB, [128, 128], name="S%d" % h) for h in range(4)]
        Sbf = [sb(sB, [128, 128], BF16, name="Sbf%d" % h) for h in range(4)]
        P.dma('sp', out=gnw_b[:], in_=gnw_d[:, :])
        P.dma('sp', out=s5d_c[:], in_=s5d_d[:, :])
        P.dma('sp', out=glub_c[:], in_=glub_d[:, :])
        pT0 = ps(sB, [128, 512], name="pT0")
        pT1 = ps(sB, [128, 512], name="pT1")
        pP = [ps(sB, [128, 512], name="pP%d" % i) for i in range(4)]
        pM = ps(sB, [128, 512], name="pM")
        pX = ps(sB, [128, 512], name="pX")
        pXb = pX[:].bitcast(BF16)

        with ExitStack() as s1:
            stg = [sb(s1, [128, 2560], name="stg%d" % i) for i in range(2)]
            li = [0]

            def load_cast(dst_fn, src_ap, rows_k, ncols):
                for k in range(rows_k):
                    st = stg[li[0] % 2]
                    P.dma('sp' if li[0] % 2 == 0 else 'act', out=st[:, 0:ncols], in_=src_ap[k * 128:(k + 1) * 128, :])
                    if li[0] % 2 == 0:
                        P.I('dve', 'tensor_copy', out=dst_fn(k), in_=st[:, 0:ncols])
                    else:
                        P.I('act', 'activation', out=dst_fn(k), in_=st[:, 0:ncols], func=AF.Identity)
                    li[0] += 1

            load_cast(lambda k: win_bf[:, k, :], win_d, KC, 2560)
            load_cast(lambda k: wout_bf[:, k, :], wout_d, KC, D)
            load_cast(lambda k: glu_bf[:, k, :], glu_d, 4, 512)

            hlb = sb(s1, [128, 2, 512], name="hlb")
            P.dma('sp', out=hlb[:], in_=hlb_d[:, :, :])
            P.I('dve', 'tensor_tensor', out=oml_b[:], in0=hlb[:, 0, :], in1=hlb[:, 1, :], op=ALU.subtract)
            P.I('act', 'activation', out=lb_b[:], in_=oml_b[:], func=AF.Sigmoid)
            P.I('dve', 'tensor_scalar', out=oml_b[:], in0=lb_b[:], scalar1=-1.0, scalar2=1.0, op0=ALU.mult, op1=ALU.add)

            s5sc = sb(s1, [128, 16, 3], name="s5sc")
            bpadj = [sb(s1, [128, 256], name="bpadj%d" % i) for i in range(2)]
            cpadj = [sb(s1, [128, 256], name="cpadj%d" % i) for i in range(2)]
            P.dma('sp', out=s5sc[:], in_=s5sc_d[:, :, :])
            nm = [0]

            def t16(name=None):
                nm[0] += 1
                return sb(s1, [128, 16], name=name or ("q16_%d" % nm[0]))

            ar, ai = s5sc[:, :, 0], s5sc[:, :, 1]
            dt_, th, mag = t16(), t16(), t16()
            P.I('act', 'activation', out=dt_[:], in_=s5sc[:, :, 2], func=AF.Exp)
            P.I('dve', 'tensor_tensor', out=th[:], in0=ai, in1=dt_[:], op=ALU.mult)
            P.I('dve', 'tensor_tensor', out=mag[:], in0=ar, in1=dt_[:], op=ALU.mult)
            P.I('act', 'activation', out=mag[:], in_=mag[:], func=AF.Exp)

            def sin_of(theta, shift):
                kf, ki, ph, m = t16(), sb(s1, [128, 16], I32), t16(), t16()
                res = t16()
                P.I('dve', 'tensor_scalar', out=ph[:], in0=theta[:], scalar1=shift, scalar2=None, op0=ALU.add)
                P.I('dve', 'tensor_scalar', out=kf[:], in0=ph[:], scalar1=1.0 / TWO_PI, scalar2=None, op0=ALU.mult)
                P.I('dve', 'tensor_copy', out=ki[:], in_=kf[:])
                P.I('dve', 'tensor_copy', out=kf[:], in_=ki[:])
                P.I('dve', 'scalar_tensor_tensor', out=ph[:], in0=kf[:], scalar=-TWO_PI, in1=ph[:], op0=ALU.mult, op1=ALU.add)
                P.I('dve', 'tensor_single_scalar', out=m[:], in_=ph[:], scalar=math.pi, op=ALU.is_gt)
                P.I('dve', 'scalar_tensor_tensor', out=ph[:], in0=m[:], scalar=-TWO_PI, in1=ph[:], op0=ALU.mult, op1=ALU.add)
                P.I('dve', 'tensor_single_scalar', out=m[:], in_=ph[:], scalar=-math.pi, op=ALU.is_lt)
                P.I('dve', 'scalar_tensor_tensor', out=ph[:], in0=m[:], scalar=TWO_PI, in1=ph[:], op0=ALU.mult, op1=ALU.add)
                P.I('act', 'activation', out=res[:], in_=ph[:], func=AF.Sin)
                return res

            sn = sin_of(th, 0.0)
            cs = sin_of(th, math.pi / 2)
            abr, abi, den, nr, cre, cim, tq = t16(), t16(), t16(), t16(), t16(), t16(), t16()
            P.I('dve', 'tensor_tensor', out=abr[:], in0=mag[:], in1=cs[:], op=ALU.mult)
            P.I('dve', 'tensor_tensor', out=abi[:], in0=mag[:], in1=sn[:], op=ALU.mult)
            P.I('dve', 'tensor_tensor', out=den[:], in0=ar, in1=ar, op=ALU.mult)
            P.I('dve', 'tensor_tensor', out=tq[:], in0=ai, in1=ai, op=ALU.mult)
            P.I('dve', 'tensor_tensor', out=den[:], in0=den[:], in1=tq[:], op=ALU.add)
            P.I('dve', 'reciprocal', out=den[:], in_=den[:])
            P.I('dve', 'tensor_scalar', out=nr[:], in0=abr[:], scalar1=-1.0, scalar2=None, op0=ALU.add)
            P.I('dve', 'tensor_tensor', out=cre[:], in0=nr[:], in1=ar, op=ALU.mult)
            P.I('dve', 'tensor_tensor', out=tq[:], in0=abi[:], in1=ai, op=ALU.mult)
            P.I('dve', 'tensor_tensor', out=cre[:], in0=cre[:], in1=tq[:], op=ALU.add)
            P.I('dve', 'tensor_tensor', out=cre[:], in0=cre[:], in1=den[:], op=ALU.mult)
            P.I('dve', 'tensor_tensor', out=cim[:], in0=abi[:], in1=ar, op=ALU.mult)
            P.I('dve', 'tensor_tensor', out=tq[:], in0=nr[:], in1=ai, op=ALU.mult)
            P.I('dve', 'tensor_tensor', out=cim[:], in0=cim[:], in1=tq[:], op=ALU.subtract)
            P.I('dve', 'tensor_tensor', out=cim[:], in0=cim[:], in1=den[:], op=ALU.mult)
            bb = sb(s1, [128, 256], name="bb")
            tb = sb(s1, [128, 128], name="tb")
            for j in range(16):
                bp, cp = bpadj[j % 2], cpadj[j % 2]
                P.dma('sp', out=bp[:], in_=bpad_d[j, :, :])
                P.dma('act', out=cp[:], in_=cpad_d[j, :, :])
                P.I('act', 'activation', out=Cb[:, j, 0:128], in_=cp[:, 0:128], func=AF.Identity)
                P.I('act', 'activation', out=Cb[:, j, 128:256], in_=cp[:, 128:256], func=AF.Identity, scale=-1.0)
                P.I('dve', 'tensor_scalar', out=tb[:], in0=bp[:, 128:256], scalar1=cim[:, j:j + 1], scalar2=None, op0=ALU.mult)
                P.I('dve', 'scalar_tensor_tensor', out=bb[:, 0:128], in0=bp[:, 0:128], scalar=cre[:, j:j + 1],
                    in1=tb[:], op0=ALU.mult, op1=ALU.subtract)
                P.I('dve', 'tensor_scalar', out=tb[:], in0=bp[:, 128:256], scalar1=cre[:, j:j + 1], scalar2=None, op0=ALU.mult)
                P.I('dve', 'scalar_tensor_tensor', out=bb[:, 128:256], in0=bp[:, 0:128], scalar=cim[:, j:j + 1],
                    in1=tb[:], op0=ALU.mult, op1=ALU.add)
                P.I('pe', 'transpose', out=pT0[:, 0:128], in_=bb[:, 0:128], identity=ident_f[:])
                P.I('pe', 'transpose', out=pT0[:, 128:256], in_=bb[:, 128:256], identity=ident_f[:])
                P.I('act', 'activation', out=BbT[:, j, :], in_=pT0[:, 0:256], func=AF.Identity)
            P.I('dve', 'tensor_copy', out=cs_p[:], in_=cs[:])
            P.I('dve', 'tensor_copy', out=sn_p[:], in_=sn[:])
            P.I('dve', 'memset', ap=cosT[:, :, TC - 1:TC], constant=1.0)
            P.I('dve', 'memset', ap=sinT[:, :, TC - 1:TC], constant=0.0)
            d1 = sb(s1, [128, 16, TC // 2], name="dbl1")
            d2 = sb(s1, [128, 16, TC // 2], name="dbl2")
            sre, sim, tq1 = t16(), t16(), t16()

            def cmul_scalar(lo):
                P.I('dve', 'tensor_tensor', out=sre[:], in0=cosT[:, :, lo], in1=abr[:], op=ALU.mult)
                P.I('dve', 'tensor_tensor', out=tq1[:], in0=sinT[:, :, lo], in1=abi[:], op=ALU.mult)
                P.I('dve', 'tensor_tensor', out=sre[:], in0=sre[:], in1=tq1[:], op=ALU.subtract)
                P.I('dve', 'tensor_tensor', out=sim[:], in0=cosT[:, :, lo], in1=abi[:], op=ALU.mult)
                P.I('dve', 'tensor_tensor', out=tq1[:], in0=sinT[:, :, lo], in1=abr[:], op=ALU.mult)
                P.I('dve', 'tensor_tensor', out=sim[:], in0=sim[:], in1=tq1[:], op=ALU.add)
            k = 1
            while k < TC:
                lo = TC - k
                cmul_scalar(lo)
                srb = sre[:].unsqueeze(2).to_broadcast([128, 16, k])
                sib = sim[:].unsqueeze(2).to_broadcast([128, 16, k])
                P.I('dve', 'tensor_tensor', out=d1[:, :, 0:k], in0=cosT[:, :, lo:TC], in1=srb, op=ALU.mult)
                P.I('dve', 'tensor_tensor', out=d2[:, :, 0:k], in0=sinT[:, :, lo:TC], in1=sib, op=ALU.mult)
                P.I('dve', 'tensor_tensor', out=cosT[:, :, lo - k:lo], in0=d1[:, :, 0:k], in1=d2[:, :, 0:k], op=ALU.subtract)
                P.I('dve', 'tensor_tensor', out=d1[:, :, 0:k], in0=cosT[:, :, lo:TC], in1=sib, op=ALU.mult)
                P.I('dve', 'tensor_tensor', out=d2[:, :, 0:k], in0=sinT[:, :, lo:TC], in1=srb, op=ALU.mult)
                P.I('dve', 'tensor_tensor', out=sinT[:, :, lo - k:lo], in0=d1[:, :, 0:k], in1=d2[:, :, 0:k], op=ALU.add)
                k *= 2
            cmul_scalar(0)
            P.I('dve', 'tensor_copy', out=A256r[:], in_=sre[:])
            P.I('dve', 'tensor_copy', out=A256i[:], in_=sim[:])
            P.I('dve', 'tensor_copy', out=mag_p[:], in_=mag[:])
            P.I('dve', 'memset', ap=car_re[:], constant=0.0)
            P.I('dve', 'memset', ap=car_im[:], constant=0.0)
            for h in range(4):
                P.I('dve', 'memset', ap=Sst[h][:], constant=0.0)
                P.I('dve', 'memset', ap=Sbf[h][:], constant=0.0)
        P.barrier()

        with ExitStack() as s2:
            xt = [sb(s2, [128, D], name="xt%d" % i) for i in range(2)]
            xn = sb(s2, [128, D], name="xn")
            ss = sb(s2, [128, 1], name="ss")
            hTs = [sb(s2, [128, KC, 512], BF16, name="hT%d" % i) for i in range(2)]
            uTs = [sb(s2, [128, 4, 512], BF16, name="uT%d" % i) for i in range(2)]
            du = sb(s2, [128, 4, 512], name="du")
            fsig = sb(s2, [128, 512], name="fsig")
            fv = fsig
            logf = sb(s2, [128, 512], name="logf")
            omf = sb(s2, [128, 512], name="omf")
            v_tm = sb(s2, [128, 512], BF16, name="v_tm")
            qs = sb(s2, [128, 512], name="qs")
            sg = sb(s2, [128, 512], name="sg")
            ep = sb(s2, [128, 128], name="ep")
            em = sb(s2, [128, 128], name="em")
            el = sb(s2, [128, 128], name="el")
            eb = sb(s2, [128, 2], name="eb")
            qd = sb(s2, [128, 128], BF16, name="qd")
            kd = sb(s2, [128, 128], BF16, name="kd")
            kdl = sb(s2, [128, 128], BF16, name="kdl")
            qkT = sb(s2, [128, 256], BF16, name="qkT")
            scm = sb(s2, [128, 128], BF16, name="scm")
            osq = sb(s2, [128, 1], name="osq")
            ojunk = sb(s2, [128, 128], name="ojunk")
            on = sb(s2, [128, 128], name="on")
            ob = sb(s2, [128, 128], BF16, name="ob")
            ohT = sb(s2, [128, 4, 512], BF16, name="ohT")
            s5t = [sb(s2, [128, TC], name="s5t%d" % i) for i in range(4)]
            wre = sb(s2, [128, TC], name="wre")
            wim = sb(s2, [128, TC], name="wim")
            rho_j = sb(s2, [128, TC], name="rho_j")
            tc1 = sb(s2, [128, 1], name="tc1")
            tc2 = sb(s2, [128, 1], name="tc2")
            xre = sb(s2, [128, 512], BF16, name="xre")
            xim = sb(s2, [128, 512], BF16, name="xim")
            yv = sb(s2, [128, 512], name="yv")
            yg = sb(s2, [128, 4, 512], BF16, name="yg")
            os5T = sb(s2, [128, 4, 512], BF16, name="os5T")
            acc = sb(s2, [128, 16, 8], name="acc")
            px = [s5t[2], s5t[3]]
            zre = sb(s2, [128, TC], name="zre")
            zim = sb(s2, [128, TC], name="zim")
            cq = [sb(s2, [128, 16], name="cq%d" % i) for i in range(4)]
            x1tmp = yv

            def is_own(blk):
                return blk // BPS == 3

            def emit_B1_tile(nb, tt):
                slot = nb // BPS
                hT = hTs[nb % 2]
                x_ = xt[tt % 2]
                pos = nb * 512 + tt * 128
                P.dma('sp', out=x_[:], in_=xin[pos:pos + 128, :])
                P.I('act', 'activation', out=xn[:], in_=x_[:], func=AF.Square, accum_out=ss[:])
                P.I('dve', 'tensor_scalar', out=ss[:], in0=ss[:], scalar1=1.0 / D, scalar2=EPS, op0=ALU.mult, op1=ALU.add)
                P.I('act', 'activation', out=ss[:], in_=ss[:], func=AF.Ln)
                P.I('act', 'activation', out=ss[:], in_=ss[:], func=AF.Exp, scale=-0.5)
                P.I('dve', 'tensor_scalar', out=xn[:], in0=x_[:], scalar1=ss[:, 0:1], scalar2=None, op0=ALU.mult)
                for k in range(KC):
                    pt = pT1 if k < 4 else pX
                    P.I('pe', 'transpose', out=pt[:, (k % 4) * 128:(k % 4 + 1) * 128], in_=xn[:, k * 128:(k + 1) * 128],
                        identity=ident_f[:])
                for k in range(KC):
                    pt = pT1 if k < 4 else pX
                    src = pt[:, (k % 4) * 128:(k % 4 + 1) * 128]
                    dst = hT[:, k, tt * 128:(tt + 1) * 128]
                    if k < 4:
                        P.I('act', 'activation', out=dst, in_=src, func=AF.Identity,
                            scale=scale1m[:, slot, k:k + 1], bias=bias1m[:, slot, k:k + 1])
                    else:
                        P.I('dve', 'tensor_scalar', out=dst, in0=src, scalar1=scale1m[:, slot, k:k + 1],
                            scalar2=bias1m[:, slot, k:k + 1], op0=ALU.mult, op1=ALU.add)

            def emit_B2_cg(nb, cg):
                hT, uT = hTs[nb % 2], uTs[nb % 2]
                pt = pT1 if cg % 2 == 0 else pX
                for k in range(KC):
                    P.I('pe', 'matmul', out=pt[:, 0:512], lhsT=win_bf[:, k, 2048 + cg * 128:2048 + (cg + 1) * 128],
                        rhs=hT[:, k, :], start=(k == 0), stop=(k == KC - 1))
                P.I('act', 'activation', out=uT[:, cg, :], in_=pt[:, 0:512], func=AF.Identity)
                if is_own(nb):
                    P.I('dve', 'tensor_scalar', out=du[:, cg, :], in0=pt[:, 0:512], scalar1=s5d_c[:, cg:cg + 1],
                        scalar2=None, op0=ALU.mult)

            def hg_tile(blk, tt, own):
                hT = hTs[blk % 2]
                sl = slice(tt * 128, (tt + 1) * 128)
                fams = [(0, 512), (1, 1024)] + ([(2, 0), (3, 1536)] if own else [])
                for (pi, c0) in fams:
                    for k in range(KC):
                        P.I('pe', 'matmul', out=pP[pi][:, 0:512], lhsT=hT[:, k, sl], rhs=win_bf[:, k, c0:c0 + 512],
                            start=(k == 0), stop=(k == KC - 1))
                P.I('act', 'activation', out=fsig[:], in_=pP[0][:, 0:512], func=AF.Sigmoid)
                P.I('dve', 'tensor_tensor', out=fv[:], in0=fsig[:], in1=oml_b[:], op=ALU.mult)
                P.I('dve', 'tensor_tensor', out=fv[:], in0=fv[:], in1=lb_b[:], op=ALU.add)
                P.I('act', 'activation', out=logf[:], in_=fv[:], func=AF.Ln)
                P.I('dve', 'tensor_scalar', out=omf[:], in0=fv[:], scalar1=-1.0, scalar2=1.0, op0=ALU.mult, op1=ALU.add)
                P.I('act', 'activation', out=v_tm[:], in_=pP[1][:, 0:512], func=AF.Identity)
                if own:
                    P.I('act', 'activation', out=qs[:], in_=pP[2][:, 0:512], func=AF.Silu)
                    P.I('act', 'activation', out=sg[:], in_=pP[3][:, 0:512], func=AF.Silu)

            def hg_unit(blk, tt, hd, own):
                sl = slice(tt * 128, (tt + 1) * 128)
                c = slice(hd * 128, (hd + 1) * 128)
                P.I('pe', 'matmul', out=pM[:, 0:128], lhsT=tri2[:], rhs=logf[:, c], start=True, stop=True)
                P.I('pe', 'matmul', out=pM[:, 128:256], lhsT=suf2[:], rhs=logf[:, c], start=True, stop=True)
                P.I('pe', 'matmul', out=pM[:, 384:386], lhsT=logf[:, c], rhs=chunkind[:], start=True, stop=True)
                if own:
                    P.I('act', 'activation', out=ep[:], in_=pM[:, 0:128], func=AF.Exp)
                    P.I('act', 'activation', out=em[:], in_=pM[:, 0:128], func=AF.Exp, scale=-1.0)
                P.I('act', 'activation', out=el[:], in_=pM[:, 128:256], func=AF.Exp)
                P.I('act', 'activation', out=eb[:], in_=pM[:, 384:386], func=AF.Exp)
                P.I('dve', 'tensor_tensor', out=kdl[:], in0=omf[:, c], in1=el[:], op=ALU.mult)
                if own:
                    P.I('dve', 'scalar_tensor_tensor', out=qd[:], in0=qs[:, c], scalar=DK ** -0.5, in1=ep[:],
                        op0=ALU.mult, op1=ALU.mult)
                    P.I('dve', 'tensor_tensor', out=kd[:], in0=omf[:, c], in1=em[:], op=ALU.mult)
                    P.I('pe', 'transpose', out=pXb[:, 0:128], in_=qd[:], identity=ident_b[:])
                    P.I('pe', 'transpose', out=pXb[:, 128:256], in_=kd[:], identity=ident_b[:])
                    P.I('act', 'activation', out=qkT[:], in_=pXb[:, 0:256], func=AF.Identity)
                    P.I('pe', 'matmul', out=pM[:, 256:384], lhsT=qkT[:, 128:256], rhs=qkT[:, 0:128], start=True, stop=True)
                    P.I('dve', 'tensor_tensor', out=scm[:], in0=pM[:, 256:384], in1=tri2[:], op=ALU.mult)
                    P.I('pe', 'matmul', out=pT1[:, 0:128], lhsT=scm[:], rhs=v_tm[:, c], start=True, stop=False)
                    P.I('pe', 'matmul', out=pT1[0:64, 0:128], lhsT=qkT[:, 0:64], rhs=Sbf[hd][:], start=False, stop=False)
                for c2 in range(2):
                    r = slice(c2 * 64, (c2 + 1) * 64)
                    P.I('pe', 'matmul', out=pT0[:, 0:128], lhsT=kdl[r, :], rhs=v_tm[r, c], start=True, stop=True)
                    P.I('dve', 'scalar_tensor_tensor', out=Sst[hd][:], in0=Sst[hd][:], scalar=eb[:, c2:c2 + 1],
                        in1=pT0[:, 0:128], op0=ALU.mult, op1=ALU.add)
                    P.I('act', 'activation', out=Sbf[hd][:], in_=Sst[hd][:], func=AF.Identity)
                    if own and c2 == 0:
                        P.I('pe', 'matmul', out=pT1[64:128, 0:128], lhsT=qkT[:, 64:128], rhs=Sbf[hd][:], start=False, stop=True)
                if own:
                    P.I('act', 'activation', out=ojunk[:], in_=pT1[:, 0:128], func=AF.Square, accum_out=osq[:])
                    P.I('dve', 'tensor_scalar', out=osq[:], in0=osq[:], scalar1=1.0 / DK, scalar2=EPS, op0=ALU.mult, op1=ALU.add)
                    P.I('act', 'activation', out=osq[:], in_=osq[:], func=AF.Ln)
                    P.I('act', 'activation', out=osq[:], in_=osq[:], func=AF.Exp, scale=-0.5)
                    P.I('dve', 'scalar_tensor_tensor', out=on[:], in0=pT1[:, 0:128], scalar=osq[:, 0:1], in1=gnw_b[:],
                        op0=ALU.mult, op1=ALU.mult)
                    P.I('dve', 'tensor_tensor', out=ob[:], in0=on[:], in1=sg[:, c], op=ALU.mult)
                    P.I('pe', 'transpose', out=pXb[:, 256:384], in_=ob[:], identity=ident_b[:])
                    P.I('act', 'activation', out=ohT[:, hd, sl], in_=pXb[:, 256:384], func=AF.Identity)

            def s5_prefix_j(blk, j):
                uT = uTs[blk % 2]
                cg = j // 4
                P.I('pe', 'matmul', out=pP[2][:, 0:512], lhsT=BbT[:, j, 0:128], rhs=uT[:, cg, :], start=True, stop=True)
                P.I('pe', 'matmul', out=pP[3][:, 0:512], lhsT=BbT[:, j, 128:256], rhs=uT[:, cg, :], start=True, stop=True)
                for hf in range(2):
                    cs_ = slice(hf * TC, (hf + 1) * TC)
                    for (ci, src, tab) in ((0, pP[2], cosT), (1, pP[3], sinT), (2, pP[3], cosT), (3, pP[2], sinT)):
                        P.I('dve', 'scalar_tensor_tensor', out=s5t[0][:], in0=src[:, cs_], scalar=1.0, in1=tab[:, j, :],
                            op0=ALU.mult, op1=ALU.mult, accum_out=acc[:, j, hf * 4 + ci:hf * 4 + ci + 1])

            def s5_prefix_combine():
                for hf in range(2):
                    o = hf * 4
                    P.I('dve', 'tensor_tensor', out=cq[0][:], in0=A256r[:], in1=car_re[:], op=ALU.mult)
                    P.I('dve', 'tensor_tensor', out=cq[1][:], in0=A256i[:], in1=car_im[:], op=ALU.mult)
                    P.I('dve', 'tensor_tensor', out=cq[2][:], in0=A256r[:], in1=car_im[:], op=ALU.mult)
                    P.I('dve', 'tensor_tensor', out=cq[3][:], in0=A256i[:], in1=car_re[:], op=ALU.mult)
                    P.I('dve', 'tensor_tensor', out=car_re[:], in0=cq[0][:], in1=cq[1][:], op=ALU.subtract)
                    P.I('dve', 'tensor_tensor', out=car_im[:], in0=cq[2][:], in1=cq[3][:], op=ALU.add)
                    P.I('dve', 'tensor_tensor', out=car_re[:], in0=car_re[:], in1=acc[:, :, o + 0], op=ALU.add)
                    P.I('dve', 'tensor_tensor', out=car_re[:], in0=car_re[:], in1=acc[:, :, o + 1], op=ALU.subtract)
                    P.I('dve', 'tensor_tensor', out=car_im[:], in0=car_im[:], in1=acc[:, :, o + 2], op=ALU.add)
                    P.I('dve', 'tensor_tensor', out=car_im[:], in0=car_im[:], in1=acc[:, :, o + 3], op=ALU.add)

            def build_rot():
                P.I('dve', 'tensor_copy', out=cosT[:, :, 0], in_=cs_p[:])
                P.I('dve', 'tensor_copy', out=sinT[:, :, 0], in_=sn_p[:])
                d1 = xn[:].rearrange("p (j c) -> p j c", c=128)
                d2 = xt[0][:].rearrange("p (j c) -> p j c", c=128)
                for jh in range(2):
                    js = slice(jh * 8, (jh + 1) * 8)
                    k = 1
                    while k < TC:
                        cb_ = cosT[:, js, k - 1:k].to_broadcast([128, 8, k])
                        sb_ = sinT[:, js, k - 1:k].to_broadcast([128, 8, k])
                        P.I('dve', 'tensor_tensor', out=d1[:, :, 0:k], in0=cosT[:, js, 0:k], in1=cb_, op=ALU.mult)
                        P.I('dve', 'tensor_tensor', out=d2[:, :, 0:k], in0=sinT[:, js, 0:k], in1=sb_, op=ALU.mult)
                        P.I('dve', 'tensor_tensor', out=cosT[:, js, k:2 * k], in0=d1[:, :, 0:k], in1=d2[:, :, 0:k], op=ALU.subtract)
                        P.I('dve', 'tensor_tensor', out=d1[:, :, 0:k], in0=cosT[:, js, 0:k], in1=sb_, op=ALU.mult)
                        P.I('dve', 'tensor_tensor', out=d2[:, :, 0:k], in0=sinT[:, js, 0:k], in1=cb_, op=ALU.mult)
                        P.I('dve', 'tensor_tensor', out=sinT[:, js, k:2 * k], in0=d1[:, :, 0:k], in1=d2[:, :, 0:k], op=ALU.add)
                        k *= 2

            def s5_own_j(blk, j):
                uT = uTs[blk % 2]
                cg = j // 4
                P.I('pe', 'matmul', out=pM[:, 0:512], lhsT=BbT[:, j, 0:128], rhs=uT[:, cg, :], start=True, stop=True)
                P.I('pe', 'matmul', out=pT0[:, 0:512], lhsT=BbT[:, j, 128:256], rhs=uT[:, cg, :], start=True, stop=True)
                cosj, sinj = cosT[:, j, :], sinT[:, j, :]
                P.I('dve', 'tensor_copy', out=rho_j[:], in_=mag_p[:, j:j + 1].to_broadcast([128, TC]))
                for hf in range(2):
                    cs_ = slice(hf * TC, (hf + 1) * TC)
                    P.I('dve', 'tensor_tensor', out=s5t[0][:], in0=pM[:, cs_], in1=cosj, op=ALU.mult)
                    P.I('dve', 'tensor_tensor', out=s5t[1][:], in0=pT0[:, cs_], in1=sinj, op=ALU.mult)
                    P.I('dve', 'tensor_tensor', out=wre[:], in0=s5t[0][:], in1=s5t[1][:], op=ALU.add)
                    P.I('dve', 'tensor_tensor', out=s5t[0][:], in0=pT0[:, cs_], in1=cosj, op=ALU.mult)
                    P.I('dve', 'tensor_tensor', out=s5t[1][:], in0=pM[:, cs_], in1=sinj, op=ALU.mult)
                    P.I('dve', 'tensor_tensor', out=wim[:], in0=s5t[0][:], in1=s5t[1][:], op=ALU.subtract)
                    P.I('dve', 'tensor_tensor_scan', out=zre[:], data0=rho_j[:], data1=wre[:],
                        initial=car_re[:, j:j + 1], op0=ALU.mult, op1=ALU.add)
                    P.I('dve', 'tensor_tensor_scan', out=zim[:], data0=rho_j[:], data1=wim[:],
                        initial=car_im[:, j:j + 1], op0=ALU.mult, op1=ALU.add)
                    L = TC - 1
                    P.I('dve', 'tensor_scalar', out=tc1[:], in0=zim[:, L:L + 1], scalar1=sinT[:, j, L:L + 1], scalar2=None, op0=ALU.mult)
                    P.I('dve', 'tensor_scalar', out=tc2[:], in0=zim[:, L:L + 1], scalar1=cosT[:, j, L:L + 1], scalar2=None, op0=ALU.mult)
                    P.I('dve', 'scalar_tensor_tensor', out=car_re[:, j:j + 1], in0=zre[:, L:L + 1], scalar=cosT[:, j, L:L + 1],
                        in1=tc1[:], op0=ALU.mult, op1=ALU.subtract)
                    P.I('dve', 'scalar_tensor_tensor', out=car_im[:, j:j + 1], in0=zre[:, L:L + 1], scalar=sinT[:, j, L:L + 1],
                        in1=tc2[:], op0=ALU.mult, op1=ALU.add)
                    P.I('pool', 'tensor_tensor', out=px[0][:], in0=zre[:], in1=cosj, op=ALU.mult)
                    P.I('pool', 'tensor_tensor', out=px[1][:], in0=zim[:], in1=sinj, op=ALU.mult)
                    P.I('pool', 'tensor_tensor', out=xre[:, cs_], in0=px[0][:], in1=px[1][:], op=ALU.subtract)
                    P.I('pool', 'tensor_tensor', out=px[0][:], in0=zre[:], in1=sinj, op=ALU.mult)
                    P.I('pool', 'tensor_tensor', out=px[1][:], in0=zim[:], in1=cosj, op=ALU.mult)
                    P.I('pool', 'tensor_tensor', out=xim[:, cs_], in0=px[0][:], in1=px[1][:], op=ALU.add)
                P.I('pe', 'matmul', out=pP[cg][:, 0:512], lhsT=Cb[:, j, 0:128], rhs=xre[:], start=(j % 4 == 0), stop=False)
                P.I('pe', 'matmul', out=pP[cg][:, 0:512], lhsT=Cb[:, j, 128:256], rhs=xim[:], start=False, stop=(j % 4 == 3))

            def own_tail(blk):
                for cg in range(4):
                    P.I('dve', 'tensor_tensor', out=yv[:], in0=pP[cg][:, 0:512], in1=du[:, cg, :], op=ALU.add)
                    P.I('act', 'activation', out=yg[:, cg, :], in_=yv[:], func=AF.Gelu)
                for co in range(4):
                    for k in range(4):
                        P.I('pe', 'matmul', out=pP[co][:, 0:512], lhsT=glu_bf[:, k, co * 128:(co + 1) * 128], rhs=yg[:, k, :],
                            start=(k == 0), stop=(k == 3))
                    P.I('act', 'activation', out=yv[:], in_=pP[co][:, 0:512], func=AF.Sigmoid, bias=glub_c[:, co:co + 1])
                    P.I('dve', 'tensor_tensor', out=os5T[:, co, :], in0=yg[:, co, :], in1=yv[:], op=ALU.mult)
                xres = xt[0]
                for tt in range(4):
                    sl = slice(tt * 128, (tt + 1) * 128)
                    pos = blk * 512 + tt * 128
                    opos = pos - 3 * SEG
                    P.dma('sp', out=xres[:], in_=xin[pos:pos + 128, :])
                    for hf in range(2):
                        for k in range(KC):
                            lh = ohT[:, k, sl] if k < 4 else os5T[:, k - 4, sl]
                            P.I('pe', 'matmul', out=pP[hf][:, 0:512], lhsT=lh, rhs=wout_bf[:, k, hf * 512:(hf + 1) * 512],
                                start=(k == 0), stop=(k == KC - 1))
                        P.I('dve', 'tensor_tensor', out=x1tmp[:], in0=pP[hf][:, 0:512], in1=gt1_b[:, hf * 512:(hf + 1) * 512], op=ALU.mult)
                        P.I('dve', 'tensor_tensor', out=xres[:, hf * 512:(hf + 1) * 512], in0=x1tmp[:], in1=xres[:, hf * 512:(hf + 1) * 512], op=ALU.add)
                    P.dma('sp', out=x1s[opos:opos + 128, :], in_=xres[:])

            for tt in range(4):
                emit_B1_tile(0, tt)
            for cg in range(4):
                emit_B2_cg(0, cg)
            for blk in range(NBLK):
                own = is_own(blk)
                nb = blk + 1
                has_next = nb < NBLK
                if not own:
                    for i in range(16):
                        tt, hd = i // 4, i % 4
                        if has_next and hd == 0:
                            emit_B1_tile(nb, tt)
                        if hd == 0:
                            hg_tile(blk, tt, False)
                        hg_unit(blk, tt, hd, False)
                        s5_prefix_j(blk, i)
                        if has_next and i >= 13:
                            emit_B2_cg(nb, i - 13)
                    if has_next:
                        emit_B2_cg(nb, 3)
                    s5_prefix_combine()
                else:
                    if blk == 3 * BPS:
                        build_rot()
                    for tt in range(4):
                        hg_tile(blk, tt, True)
                        for hd in range(4):
                            hg_unit(blk, tt, hd, True)
                    for j in range(16):
                        s5_own_j(blk, j)
                    own_tail(blk)
                    if has_next:
                        for tt in range(4):
                            emit_B1_tile(nb, tt)
                        for cg in range(4):
                            emit_B2_cg(nb, cg)
        P.barrier()

    if "C" in phases:
      with ExitStack() as sC:
        wq_bf = sb(sC, [128, KC, 2048], BF16, name="wq_bf")
        keys_bf = sb(sC, [128, 256], BF16, name="keys_bf")
        gt2_b = sb(sC, [128, D], name="gt2_b")
        sh2_b = sb(sC, [128, D], name="sh2_b")
        sc2_b = sb(sC, [128, D], name="sc2_b")
        fnw_b = sb(sC, [128, D], name="fnw_bs")
        iota16 = sb(sC, [128, 16], name="iota16s")
        cA = [ps(sC, [128, 1024], name="cA%d" % i) for i in range(2)]
        cB = [ps(sC, [128, 1024], name="cB%d" % i) for i in range(2)]
        P.dma('sp', out=sh2_b[:], in_=bcs[0, :, :])
        P.dma('sp', out=sc2_b[:], in_=bcs[1, :, :])
        P.dma('sp', out=gt2_b[:], in_=bcs[2, :, :])
        P.dma('sp', out=fnw_b[:], in_=fnw_d[:, :])
        P.dma('sp', out=iota16[:], in_=iota16_d[:, :])
        with ExitStack() as s1:
            stg = [sb(s1, [128, 2048], name="stgc%d" % i) for i in range(2)]
            for k in range(KC):
                st = stg[k % 2]
                P.dma('sp' if k % 2 == 0 else 'act', out=st[:], in_=wq_d[k * 128:(k + 1) * 128, :])
                if k % 2 == 0:
                    P.I('dve', 'tensor_copy', out=wq_bf[:, k, :], in_=st[:])
                else:
                    P.I('act', 'activation', out=wq_bf[:, k, :], in_=st[:], func=AF.Identity)
            P.dma('sp', out=stg[0][:, 0:256], in_=keysT_d[:, :])
            P.I('dve', 'tensor_copy', out=keys_bf[:], in_=stg[0][:, 0:256])
        P.barrier()
        with ExitStack() as s2:
            x1ts = [sb(s2, [128, D], name="cx1t%d" % i) for i in range(2)]
            h2tms = [sb(s2, [128, D], BF16, name="ch2tm%d" % i) for i in range(2)]
            eidTs = [sb(s2, [128, 128], U32, name="ceidT%d" % i) for i in range(2)]
            gatesTs = [sb(s2, [128, 128], name="cgatesT%d" % i) for i in range(2)]
            xn2 = sb(s2, [128, D], name="cxn2")
            ss = sb(s2, [128, 1], name="css")
            ss2 = sb(s2, [128, 1], name="css2")
            h2f = sb(s2, [128, D], name="ch2f")
            x2f = sb(s2, [128, D], name="cx2f")
            h2T = sb(s2, [128, KC, 128], BF16, name="ch2T")
            qT = sb(s2, [128, 16, 128], BF16, name="cqT")
            sc = sb(s2, [128, 16, 128], name="csc")
            scw = sb(s2, [128, 16, 128], name="cscw")
            vals = sb(s2, [128, 8, 2, 16], name="cvals")
            idxu = sb(s2, [128, 8, 2, 16], U32, name="cidxu")
            idxf = sb(s2, [128, 8, 2, 16], name="cidxf")
            cand = sb(s2, [128, 8, 256], name="ccand")
            candw = sb(s2, [128, 8, 256], name="ccandw")
            best = sb(s2, [128, 8, 16], name="cbest")
            posu = sb(s2, [128, 8, 16], U32, name="cposu")
            piu = sb(s2, [128, 8, 16], U32, name="cpiu")
            pju = sb(s2, [128, 8, 16], U32, name="cpju")
            pif = sb(s2, [128, 8, 16], name="cpif")
            pjf = sb(s2, [128, 8, 16], name="cpjf")
            oh = sb(s2, [128, 8, 16, 16], name="coh")
            sel1 = sb(s2, [128, 8, 16], name="csel1")
            sel2 = sb(s2, [128, 8, 16], name="csel2")
            eidf = sb(s2, [128, 128], name="ceidf")
            gd = sb(s2, [128, 8, 16], name="cgd")
            gsum = sb(s2, [128, 8], name="cgsum")
            gates = sb(s2, [128, 128], name="cgates")
            NB = 8
            acol = [sb(s2, [128, 2], name="cacol%d" % i) for i in range(NB)]
            agcol = [sb(s2, [128, 2], name="cagcol%d" % i) for i in range(NB)]
            wcol = [sb(s2, [128, 2], BF16, name="cwcol%d" % i) for i in range(NB)]
            UV = [sb(s2, [128, 2 * D], BF16, name="cUV%d" % i) for i in range(NB)]
            junk = sb(s2, [128, D], BF16, name="cjunk")
            peerS = sb(s2, [128, D], name="cpeerS")
            ot = sb(s2, [128, D], name="cot")
            NEG = -1.0e30
            B4 = [128, 8, 16, 16]
            cR = cB[1]
            hbufs = [cA[0], cA[1]]

            def routing_ops(it):
                par = it % 2
                x1t, h2tm, eidT, gatesT = x1ts[par], h2tms[par], eidTs[par], gatesTs[par]
                tok0 = it * 128
                ops = []
                A = ops.append
                A(lambda: P.dma('sp', out=x1t[:], in_=x1s[tok0:tok0 + 128, :]))
                A(lambda: P.I('act', 'activation', out=xn2[:], in_=x1t[:], func=AF.Square, accum_out=ss[:]))
                A(lambda: P.I('dve', 'tensor_scalar', out=ss[:], in0=ss[:], scalar1=1.0 / D, scalar2=EPS, op0=ALU.mult, op1=ALU.add))
                A(lambda: P.I('act', 'activation', out=ss[:], in_=ss[:], func=AF.Ln))
                A(lambda: P.I('act', 'activation', out=ss[:], in_=ss[:], func=AF.Exp, scale=-0.5))
                A(lambda: P.I('dve', 'tensor_scalar', out=xn2[:], in0=x1t[:], scalar1=ss[:, 0:1], scalar2=None, op0=ALU.mult))
                A(lambda: P.I('dve', 'tensor_tensor', out=h2f[:], in0=xn2[:], in1=sc2_b[:], op=ALU.mult))
                A(lambda: P.I('dve', 'tensor_tensor', out=h2tm[:], in0=h2f[:], in1=sh2_b[:], op=ALU.add))
                for k in range(KC):
                    A(lambda k=k: P.I('pe', 'transpose', out=cR[:, k * 128:(k + 1) * 128], in_=xn2[:, k * 128:(k + 1) * 128], identity=ident_f[:]))
                for k in range(KC):
                    if k % 2 == 0:
                        A(lambda k=k: P.I('act', 'activation', out=h2T[:, k, :], in_=cR[:, k * 128:(k + 1) * 128], func=AF.Identity,
                                          scale=scale2c[:, k:k + 1], bias=sh2c[:, k:k + 1]))
                    else:
                        A(lambda k=k: P.I('dve', 'tensor_scalar', out=h2T[:, k, :], in0=cR[:, k * 128:(k + 1) * 128], scalar1=scale2c[:, k:k + 1],
                                          scalar2=sh2c[:, k:k + 1], op0=ALU.mult, op1=ALU.add))
                for half in range(2):
                    for b8 in range(8):
                        b16 = half * 8 + b8
                        for k in range(KC):
                            A(lambda b16=b16, b8=b8, k=k: P.I('pe', 'matmul', out=cR[:, b8 * 128:(b8 + 1) * 128],
                                                              lhsT=wq_bf[:, k, b16 * 128:(b16 + 1) * 128], rhs=h2T[:, k, :],
                                                              start=(k == 0), stop=(k == KC - 1)))
                    A(lambda half=half: P.I('act', 'activation', out=qT[:, half * 8:(half + 1) * 8, :],
                                            in_=cR[:].rearrange("p (b n) -> p b n", n=128), func=AF.Identity))
                for half in range(2):
                    for b8 in range(8):
                        b16 = half * 8 + b8
                        A(lambda b16=b16, b8=b8: P.I('pe', 'matmul', out=cR[:, b8 * 128:(b8 + 1) * 128], lhsT=qT[:, b16, :],
                                                     rhs=keys_bf[:, (b16 % 2) * 128:(b16 % 2 + 1) * 128], start=True, stop=True))
                    A(lambda half=half: P.I('act', 'activation', out=sc[:, half * 8:(half + 1) * 8, :],
                                            in_=cR[:].rearrange("p (b n) -> p b n", n=128), func=AF.Identity))
                for b16 in range(16):
                    hd, hf = b16 // 2, b16 % 2
                    A(lambda b16=b16, hd=hd, hf=hf: P.I('dve', 'max', out=vals[:, hd, hf, 0:8], in_=sc[:, b16, :]))
                    A(lambda b16=b16, hd=hd, hf=hf: P.I('dve', 'max_index', out=idxu[:, hd, hf, 0:8], in_max=vals[:, hd, hf, 0:8], in_values=sc[:, b16, :]))
                    A(lambda b16=b16, hd=hd, hf=hf: P.I('dve', 'match_replace', out=scw[:, b16, :], in_to_replace=vals[:, hd, hf, 0:8],
                                                        in_values=sc[:, b16, :], imm_value=NEG))
                    A(lambda b16=b16, hd=hd, hf=hf: P.I('dve', 'max', out=vals[:, hd, hf, 8:16], in_=scw[:, b16, :]))
                    A(lambda b16=b16, hd=hd, hf=hf: P.I('dve', 'max_index', out=idxu[:, hd, hf, 8:16], in_max=vals[:, hd, hf, 8:16], in_values=scw[:, b16, :]))
                A(lambda: P.I('dve', 'tensor_copy', out=idxf[:], in_=idxu[:]))
                A(lambda: P.I('dve', 'tensor_tensor', out=cand[:].rearrange("p h (i j) -> p h i j", j=16),
                              in0=vals[:, :, 0, :].unsqueeze(3).to_broadcast(B4), in1=vals[:, :, 1, :].unsqueeze(2).to_broadcast(B4), op=ALU.add))
                for hd in range(8):
                    A(lambda hd=hd: P.I('dve', 'max', out=best[:, hd, 0:8], in_=cand[:, hd, :]))
                    A(lambda hd=hd: P.I('dve', 'max_index', out=posu[:, hd, 0:8], in_max=best[:, hd, 0:8], in_values=cand[:, hd, :]))
                    A(lambda hd=hd: P.I('dve', 'match_replace', out=candw[:, hd, :], in_to_replace=best[:, hd, 0:8], in_values=cand[:, hd, :], imm_value=NEG))
                    A(lambda hd=hd: P.I('dve', 'max', out=best[:, hd, 8:16], in_=candw[:, hd, :]))
                    A(lambda hd=hd: P.I('dve', 'max_index', out=posu[:, hd, 8:16], in_max=best[:, hd, 8:16], in_values=candw[:, hd, :]))
                A(lambda: P.I('dve', 'tensor_single_scalar', out=piu[:], in_=posu[:], scalar=4, op=ALU.logical_shift_right))
                A(lambda: P.I('dve', 'tensor_single_scalar', out=pju[:], in_=posu[:], scalar=15, op=ALU.bitwise_and))
                A(lambda: P.I('dve', 'tensor_copy', out=pif[:], in_=piu[:]))
                A(lambda: P.I('dve', 'tensor_copy', out=pjf[:], in_=pju[:]))
                io_b = iota16[:].unsqueeze(1).unsqueeze(1).to_broadcast(B4)
                for (pf, hf, sel) in ((pif, 0, sel1), (pjf, 1, sel2)):
                    A(lambda pf=pf: P.I('dve', 'tensor_tensor', out=oh[:], in0=pf[:].unsqueeze(3).to_broadcast(B4), in1=io_b, op=ALU.is_equal))
                    A(lambda hf=hf: P.I('dve', 'tensor_tensor', out=oh[:], in0=oh[:], in1=idxf[:, :, hf, :].unsqueeze(2).to_broadcast(B4), op=ALU.mult))
                    A(lambda sel=sel: P.I('dve', 'tensor_reduce', out=sel[:], in_=oh[:], axis=AX.X, op=ALU.add))
                A(lambda: P.I('dve', 'scalar_tensor_tensor', out=eidf[:].rearrange("p (h k) -> p h k", k=16), in0=sel1[:], scalar=128.0, in1=sel2[:],
                              op0=ALU.mult, op1=ALU.add))
                A(lambda: P.I('dve', 'tensor_tensor', out=gd[:], in0=best[:], in1=best[:, :, 0:1].to_broadcast([128, 8, 16]), op=ALU.subtract))
                A(lambda: P.I('act', 'activation', out=gd[:], in_=gd[:], func=AF.Exp))
                A(lambda: P.I('dve', 'tensor_reduce', out=gsum[:], in_=gd[:], axis=AX.X, op=ALU.add))
                A(lambda: P.I('dve', 'reciprocal', out=gsum[:], in_=gsum[:]))
                A(lambda: P.I('dve', 'tensor_tensor', out=gates[:].rearrange("p (h k) -> p h k", k=16), in0=gd[:],
                              in1=gsum[:].unsqueeze(2).to_broadcast([128, 8, 16]), op=ALU.mult))
                A(lambda: P.I('pe', 'transpose', out=cR[:, 0:128], in_=eidf[:], identity=ident_f[:]))
                A(lambda: P.I('pe', 'transpose', out=cR[:, 128:256], in_=gates[:], identity=ident_f[:]))
                A(lambda: P.I('dve', 'tensor_copy', out=eidT[:], in_=cR[:, 0:128]))
                A(lambda: P.I('act', 'activation', out=gatesT[:], in_=cR[:, 128:256], func=AF.Identity))
                return ops

            def epilogue_ops(it):
                x1t = x1ts[it % 2]
                tok0 = it * 128
                ops = []
                A = ops.append
                A(lambda: P.I('act', 'activation', out=peerS[:], in_=cB[0][:], func=AF.Identity))
                for dk in range(KC):
                    A(lambda dk=dk: P.I('pe', 'transpose', out=cR[:, dk * 128:(dk + 1) * 128], in_=peerS[:, dk * 128:(dk + 1) * 128], identity=ident_f[:]))
                A(lambda: P.I('dve', 'tensor_tensor', out=x2f[:], in0=cR[:], in1=gt2_b[:], op=ALU.mult))
                A(lambda: P.I('dve', 'tensor_tensor', out=x2f[:], in0=x2f[:], in1=x1t[:], op=ALU.add))
                A(lambda: P.I('act', 'activation', out=ot[:], in_=x2f[:], func=AF.Square, accum_out=ss2[:]))
                A(lambda: P.I('dve', 'tensor_scalar', out=ss2[:], in0=ss2[:], scalar1=1.0 / D, scalar2=EPS, op0=ALU.mult, op1=ALU.add))
                A(lambda: P.I('act', 'activation', out=ss2[:], in_=ss2[:], func=AF.Ln))
                A(lambda: P.I('act', 'activation', out=ss2[:], in_=ss2[:], func=AF.Exp, scale=-0.5))
                A(lambda: P.I('dve', 'scalar_tensor_tensor', out=ot[:], in0=x2f[:], scalar=ss2[:, 0:1], in1=fnw_b[:], op0=ALU.mult, op1=ALU.mult))
                A(lambda: P.dma('sp', out=out_d[tok0:tok0 + 128, :], in_=ot[:]))
                return ops

            def token_loop(it, side_ops):
                eidT, gatesT, h2tm = eidTs[it % 2], gatesTs[it % 2], h2tms[it % 2]
                nside = len(side_ops)
                done = [0]

                def pump(upto):
                    while done[0] < min(upto, nside):
                        side_ops[done[0]]()
                        done[0] += 1

                def tail(t):
                    r = t % NB
                    P.I('act', 'activation', out=wcol[r][:, 0:1], in_=agcol[r][:, 0:1], func=AF.Identity, scale=gatesT[:, t:t + 1])
                    for dk in range(KC):
                        P.I('pe', 'matmul', out=cB[0][:, dk * 128 + t:dk * 128 + t + 1], lhsT=UV[r][:, D + dk * 128:D + (dk + 1) * 128],
                            rhs=wcol[r][:, 0:1], start=True, stop=True)
                pump(1)
                for t in range(128 + 2):
                    if t < 128:
                        r = t % NB
                        P.gather(UV[r][:], uvb[:, :], eidT[:, t:t + 1])
                        hb = hbufs[t % 2]
                        for hh in range(2):
                            P.I('pe', 'matmul', out=hb[:, hh * 512:(hh + 1) * 512], lhsT=ident_b[:, t:t + 1].to_broadcast([128, 128]),
                                rhs=h2tm[:, hh * 512:(hh + 1) * 512], start=True, stop=True)
                    if t >= 2:
                        tail(t - 2)
                    if t < 128:
                        P.I('dve', 'scalar_tensor_tensor', out=junk[:], in0=UV[r][:, 0:D], scalar=1.0, in1=hb[:], op0=ALU.mult, op1=ALU.mult,
                            accum_out=acol[r][:, 0:1])
                        P.I('act', 'activation', out=agcol[r][:, 0:1], in_=acol[r][:, 0:1], func=AF.Gelu)
                    pump(1 + ((t + 1) * nside) // 120)
                pump(nside)

            for op_ in routing_ops(0):
                op_()
            for it in range(NT):
                side = []
                if it > 0:
                    side += epilogue_ops(it - 1)
                if it + 1 < NT:
                    side += routing_ops(it + 1)
                token_loop(it, side)
            for op_ in epilogue_ops(NT - 1):
                op_()
        P.barrier()

    P.finish()
    print("instructions:", P.ninst)
    return nc


def prep(inputs, SEG):
    f = lambda a: np.ascontiguousarray(np.asarray(a, dtype=np.float32))
    x = f(inputs['x'])
    B, S, _ = x.shape
    assert S == 4 * SEG
    col8 = lambda v: f(v.reshape(8, 128).T)
    shared = {}
    shared['ada_w'] = f(inputs['ada_w'][0])
    ab = f(inputs['ada_b'][0])
    shared['ada_bc'] = f(ab.reshape(48, 128).T)
    shared['ada_br'] = f(ab.reshape(1, -1))
    shared['nmw_c'] = col8(f(inputs['norm_mix_w'][0]))
    shared['nfw_c'] = col8(f(inputs['norm_ffn_w'][0]))
    shared['nfw_r'] = f(inputs['norm_ffn_w'][0]).reshape(1, D)
    shared['fnw_b'] = f(np.broadcast_to(f(inputs['final_norm_w']).reshape(1, D), (128, D)))
    shared['w_in'] = f(inputs['w_in'][0])
    shared['w_out'] = f(inputs['w_out'][0])
    shared['wq'] = f(inputs['peer_wq'][0])
    shared['glu_w'] = f(inputs['s5_glu_w'][0])
    shared['hlb_b'] = f(np.broadcast_to(f(inputs['hg_lower_bounds']).reshape(1, 2, 512), (128, 2, 512)))
    shared['gnw_b'] = f(np.broadcast_to(f(inputs['hg_gnorm_w'][0]).reshape(1, 128), (128, 128)))
    a_re, a_im, ldt = f(inputs['s5_a_re'][0]), f(inputs['s5_a_im'][0]), f(inputs['s5_log_dt'][0])
    s5sc = np.zeros((128, 16, 3), np.float32)
    s5sc[:, :, 0] = a_re.reshape(16, 128).T
    s5sc[:, :, 1] = a_im.reshape(16, 128).T
    s5sc[:, :, 2] = np.repeat(ldt, 64).reshape(16, 128).T
    shared['s5sc'] = s5sc
    b_re, b_im = f(inputs['s5_b_re'][0]), f(inputs['s5_b_im'][0])
    c_re, c_im = f(inputs['s5_c_re'][0]), f(inputs['s5_c_im'][0])
    bpad = np.zeros((16, 128, 256), np.float32)
    cpad = np.zeros((16, 128, 256), np.float32)
    for j in range(16):
        for g2 in range(2):
            g = 2 * j + g2
            gl = 2 * (j % 4) + g2
            bpad[j, g2 * 64:(g2 + 1) * 64, gl * 16:(gl + 1) * 16] = b_re[g]
            bpad[j, g2 * 64:(g2 + 1) * 64, 128 + gl * 16:128 + (gl + 1) * 16] = b_im[g]
            cpad[j, g2 * 64:(g2 + 1) * 64, gl * 16:(gl + 1) * 16] = c_re[g].T
            cpad[j, g2 * 64:(g2 + 1) * 64, 128 + gl * 16:128 + (gl + 1) * 16] = c_im[g].T
    shared['bpad'] = bpad
    shared['cpad'] = cpad
    shared['s5d_c'] = f(f(inputs['s5_d'][0]).reshape(4, 128).T)
    shared['glub_c'] = f(f(inputs['s5_glu_b'][0]).reshape(4, 128).T)
    shared['keysT'] = f(np.concatenate([f(inputs['peer_keys1'][0]).T, f(inputs['peer_keys2'][0]).T], axis=1))
    shared['uv_tab'] = f(np.concatenate([f(inputs['peer_u'][0]), f(inputs['peer_v'][0])], axis=1))
    shared['ident'] = np.eye(128, dtype=np.float32)
    s_i = np.arange(128)
    same = (s_i[:, None] // 64) == (s_i[None, :] // 64)
    shared['tri2'] = (same & (s_i[:, None] <= s_i[None, :])).astype(np.float32)
    shared['suf2'] = (same & (s_i[:, None] > s_i[None, :])).astype(np.float32)
    ci = np.zeros((128, 2), np.float32)
    ci[:64, 0] = 1
    ci[64:, 1] = 1
    shared['chunkind'] = ci
    shared['iota16'] = f(np.broadcast_to(np.arange(16, dtype=np.float32).reshape(1, 16), (128, 16)))
    c = f(inputs['c'])
    in_maps = []
    for r in range(8):
        b, seg = r // 4, r % 4
        m = dict(shared)
        xin = np.zeros((4 * SEG, D), np.float32)
        npre = seg * SEG
        xin[3 * SEG - npre:3 * SEG] = x[b, 0:npre]
        xin[3 * SEG:] = x[b, seg * SEG:(seg + 1) * SEG]
        m['xin'] = xin
        mk = np.zeros((128, 4), np.float32)
        mk[:, 3 - seg:] = 1.0
        m['maskc'] = mk
        m['cT'] = col8(c[b])
        in_maps.append(m)
    return in_maps


_NC_CACHE = {}


def kernel(**inputs):
    S = np.asarray(inputs['x']).shape[1]
    SEG = S // 4
    if SEG not in _NC_CACHE:
        _NC_CACHE[SEG] = build(SEG)
    nc = _NC_CACHE[SEG]
    in_maps = prep(inputs, SEG)
    res = run_bass_kernel_spmd(nc, in_maps, core_ids=list(range(8)))
    out = np.zeros((2, S, D), np.float32)
    for r in range(8):
        b, seg = r // 4, r % 4
        out[b, seg * SEG:(seg + 1) * SEG] = res.results[r]["out"]
    return out
```

```python
import math
import os
import numpy as np
from contextlib import ExitStack
import concourse.bass as bass
import concourse.mybir as mybir
from concourse.bass_utils import run_bass_kernel_spmd

F32 = mybir.dt.float32
BF16 = mybir.dt.bfloat16
I32 = mybir.dt.int32
U32 = mybir.dt.uint32
AF = mybir.ActivationFunctionType
ALU = mybir.AluOpType
AX = mybir.AxisListType

D = 1024
KC = 8
DK = 128
TC = 256
EPS = 1e-6
TWO_PI = 2.0 * math.pi


def _isap(v):
    return type(v).__name__ == 'AP'


class Prog:
    def __init__(self, nc):
        self.nc = nc
        self.es = ExitStack()
        self.eng = {'pe': nc.tensor, 'dve': nc.vector, 'act': nc.scalar, 'pool': nc.gpsimd, 'sp': nc.sync}
        self.sem = {k: self.es.enter_context(nc.semaphore("s_" + k)) for k in self.eng}
        self.cnt = {k: 0 for k in self.eng}
        self.waited = {k: {} for k in self.eng}
        self.writers = {}
        self.readers = {}
        self.dmasem = {}
        self.allmax = {}
        self.ninst = 0

    def _wait(self, e, toks):
        for (name, sem, val) in toks:
            if name == 'pe' and e == 'pe':
                continue
            if self.waited[e].get(name, 0) < val:
                self.eng[e].wait_ge(sem, val)
                self.waited[e][name] = val

    def _deps(self, e, rk, wk):
        toks = []
        for b in rk:
            toks += list(self.writers.get(b, {}).values())
        for b in wk:
            toks += list(self.writers.get(b, {}).values())
            toks += list(self.readers.get(b, {}).values())
        self._wait(e, toks)

    def _record(self, tok, rk, wk):
        for b in rk:
            self.readers.setdefault(b, {})[tok[0]] = tok
        for b in wk:
            self.writers.setdefault(b, {})[tok[0]] = tok
        self.allmax[tok[0]] = tok

    def I(self, e, meth, **kw):
        outs, ins = [], []
        for k, v in kw.items():
            if _isap(v):
                (outs if (k in ('out', 'accum_out', 'ap') or v.space == 'PSUM') else ins).append(v.name)
        self._deps(e, ins, outs)
        ins_obj = getattr(self.eng[e], meth)(**kw)
        self.cnt[e] += 1
        self.ninst += 1
        ins_obj.then_inc(self.sem[e], 1)
        self._record((e, self.sem[e], self.cnt[e]), ins, outs)

    def _dsem(self, key):
        if key not in self.dmasem:
            self.dmasem[key] = [self.es.enter_context(self.nc.semaphore("d_" + key)), 0]
        s = self.dmasem[key]
        s[1] += 16
        return s

    def dma(self, e, out, in_):
        rk, wk = [in_.name], [out.name]
        self._deps(e, rk, wk)
        key = out.name if out.space != 'DRAM' else in_.name
        s = self._dsem(key)
        self.eng[e].dma_start(out=out, in_=in_).then_inc(s[0], 16)
        self.ninst += 1
        self._record(('d_' + key, s[0], s[1]), rk, wk)

    def gather(self, out, table, idx):
        rk, wk = [table.name, idx.name], [out.name]
        self._deps('pool', rk, wk)
        s = self._dsem(out.name)
        self.nc.gpsimd.indirect_dma_start(
            out=out, out_offset=None, in_=table,
            in_offset=bass.IndirectOffsetOnAxis(ap=idx, axis=0)).then_inc(s[0], 16)
        self.ninst += 1
        self._record(('d_' + out.name, s[0], s[1]), rk, wk)

    def barrier(self):
        toks = list(self.allmax.values())
        for e in self.eng:
            self._wait(e, toks)

    def finish(self):
        self._wait('sp', list(self.allmax.values()))


def build(SEG, dbg=False, phases="ABC", lim=99):
    NPOS = 4 * SEG
    NBLK = NPOS // 512
    BPS = SEG // 512
    NT = SEG // 128
    nc = bass.Bass("TRN2", target_bir_lowering=False)

    def din(name, shape, dt=F32):
        return nc.dram_tensor(name, list(shape), dt, kind="ExternalInput").ap()

    xin = din("xin", [NPOS, D])
    maskc_d = din("maskc", [128, 4])
    cT_d = din("cT", [128, 8])
    adaw_d = din("ada_w", [D, 6 * D])
    adabc_d = din("ada_bc", [128, 48])
    adabr_d = din("ada_br", [1, 6 * D])
    nmw_d = din("nmw_c", [128, 8])
    nfw_d = din("nfw_c", [128, 8])
    nfwr_d = din("nfw_r", [1, D])
    fnw_d = din("fnw_b", [128, D])
    win_d = din("w_in", [D, 2560])
    wout_d = din("w_out", [D, D])
    wq_d = din("wq", [D, 2048])
    glu_d = din("glu_w", [512, 512])
    hlb_d = din("hlb_b", [128, 2, 512])
    gnw_d = din("gnw_b", [128, 128])
    s5sc_d = din("s5sc", [128, 16, 3])
    bpad_d = din("bpad", [16, 128, 256])
    cpad_d = din("cpad", [16, 128, 256])
    s5d_d = din("s5d_c", [128, 4])
    glub_d = din("glub_c", [128, 4])
    keysT_d = din("keysT", [128, 256])
    if "C" in phases:
        uvtab_d = din("uv_tab", [16384, 2 * D])
        uvb = nc.dram_tensor("uvb", [16384, 2 * D], BF16, kind="Internal").ap()
    ident_d = din("ident", [128, 128])
    tri2_d = din("tri2", [128, 128])
    suf2_d = din("suf2", [128, 128])
    chunkind_d = din("chunkind", [128, 2])
    iota16_d = din("iota16", [128, 16])
    out_d = nc.dram_tensor("out", [SEG, D], F32, kind="ExternalOutput").ap()
    x1s = nc.dram_tensor("x1s", [SEG, D], F32, kind="ExternalOutput" if dbg else "Internal").ap()

    P = Prog(nc)
    es = P.es
    _n = [0]

    def sb(es_, shape, dt=F32, name=None):
        _n[0] += 1
        return es_.enter_context(nc.sbuf_tensor("sb_" + (name or ("t%d" % _n[0])), list(shape), dt))

    def ps(es_, shape, dt=F32, name=None):
        _n[0] += 1
        return es_.enter_context(nc.psum_tensor("ps_" + (name or ("p%d" % _n[0])), list(shape), dt))

    ident_f = sb(es, [128, 128], name="ident_f")
    ident_b = sb(es, [128, 128], BF16, name="ident_b")
    tri2 = sb(es, [128, 128], name="tri2")
    suf2 = sb(es, [128, 128], name="suf2")
    chunkind = sb(es, [128, 2], name="chunkind")
    ones_row = sb(es, [1, 128], name="ones_row")
    gt1_b = sb(es, [128, D], name="gt1_b")
    bcs = nc.dram_tensor("bcs", [3, 128, D], F32, kind="Internal").ap()
    scale1m = sb(es, [128, 4, 8], name="scale1m")
    bias1m = sb(es, [128, 4, 8], name="bias1m")
    scale2c = sb(es, [128, 8], name="scale2c")
    sh2c = sb(es, [128, 8], name="sh2c")

    for (t, d_) in ((ident_f, ident_d), (tri2, tri2_d), (suf2, suf2_d), (chunkind, chunkind_d)):
        P.dma('sp', out=t[:], in_=d_[:, :])
    P.I('dve', 'tensor_copy', out=ident_b[:], in_=ident_f[:])
    P.I('dve', 'memset', ap=ones_row[:], constant=1.0)

    if "C" in phases:
        with ExitStack() as s0:
            cf = [sb(s0, [128, 4, 2 * D], name="castf%d" % i) for i in range(2)]
            cb16 = [sb(s0, [128, 4, 2 * D], BF16, name="castb%d" % i) for i in range(2)]
            uv_v = uvtab_d.rearrange("(c p i) d -> c p i d", p=128, i=4)
            uvb_v = uvb.rearrange("(c p i) d -> c p i d", p=128, i=4)
            for c in range(32):
                P.dma('sp' if c % 2 == 0 else 'act', out=cf[c % 2][:], in_=uv_v[c])
                e3 = ('dve', 'pool', 'act')[c % 3]
                if e3 == 'act':
                    P.I('act', 'activation', out=cb16[c % 2][:], in_=cf[c % 2][:], func=AF.Identity)
                else:
                    P.I(e3, 'tensor_copy', out=cb16[c % 2][:], in_=cf[c % 2][:])
                P.dma('sp' if c % 2 == 1 else 'act', out=uvb_v[c], in_=cb16[c % 2][:])
        P.barrier()

    with ExitStack() as sa:
        cT = sb(sa, [128, 8], name="cT")
        cond = sb(sa, [128, 8], name="cond")
        adaw = [sb(sa, [128, 8, 512], name="adaw%d" % i) for i in range(2)]
        adabc = sb(sa, [128, 48], name="adabc")
        adabr = sb(sa, [1, 6 * D], name="adabr")
        rowbuf = sb(sa, [1, 4 * D], name="rowbuf")
        nmw = sb(sa, [128, 8], name="nmw")
        nfw = sb(sa, [128, 8], name="nfw")
        nfwr = sb(sa, [1, D], name="nfwr")
        maskc = sb(sa, [128, 4], name="maskc")
        modc = sb(sa, [128, 48], name="modc")
        tmp8 = sb(sa, [128, 8], name="tmp8")
        scale1 = sb(sa, [128, 8], name="scale1")
        rtmp = sb(sa, [1, D], name="rtmp")
        modc_ps = ps(sa, [128, 512], name="modc_ps")
        row_ps = ps(sa, [128, 512], name="row_ps")
        bc_ps = ps(sa, [128, 512], name="bc_ps")

        for (t, d_) in ((cT, cT_d), (adabc, adabc_d), (adabr, adabr_d), (nmw, nmw_d), (nfw, nfw_d),
                        (nfwr, nfwr_d), (maskc, maskc_d)):
            P.dma('sp', out=t[:], in_=d_[:, :])
        P.I('act', 'activation', out=cond[:], in_=cT[:], func=AF.Silu)
        adaw_v = adaw_d.rearrange("(k p) c -> p k c", p=128)
        for jc in range(12):
            aw = adaw[jc % 2]
            P.dma('sp' if jc % 2 == 0 else 'act', out=aw[:], in_=adaw_v[:, :, jc * 512:(jc + 1) * 512])
            fam = jc // 2
            if True:
                for cb in range(4):
                    j = jc * 4 + cb
                    for k in range(KC):
                        P.I('pe', 'matmul', out=modc_ps[:, j:j + 1], lhsT=aw[:, k, cb * 128:(cb + 1) * 128],
                            rhs=cond[:, k:k + 1], start=(k == 0), stop=(k == KC - 1))
            if fam in (2, 3, 4, 5):
                for k in range(KC):
                    P.I('pe', 'matmul', out=row_ps[0:1, 0:512], lhsT=cond[:, k:k + 1], rhs=aw[:, k, :],
                        start=(k == 0), stop=(k == KC - 1))
                off = (fam - 2) * D + (jc % 2) * 512
                P.I('dve', 'tensor_tensor', out=rowbuf[0:1, off:off + 512], in0=row_ps[0:1, 0:512],
                    in1=adabr[0:1, jc * 512:(jc + 1) * 512], op=ALU.add)
        P.I('dve', 'tensor_tensor', out=modc[:], in0=modc_ps[:, 0:48], in1=adabc[:], op=ALU.add)
        P.I('dve', 'tensor_scalar', out=tmp8[:], in0=modc[:, 8:16], scalar1=1.0, scalar2=None, op0=ALU.add)
        P.I('dve', 'tensor_tensor', out=scale1[:], in0=tmp8[:], in1=nmw[:], op=ALU.mult)
        P.I('dve', 'tensor_scalar', out=tmp8[:], in0=modc[:, 32:40], scalar1=1.0, scalar2=None, op0=ALU.add)
        P.I('dve', 'tensor_tensor', out=scale2c[:], in0=tmp8[:], in1=nfw[:], op=ALU.mult)
        P.I('dve', 'tensor_copy', out=sh2c[:], in_=modc[:, 24:32])
        for s in range(4):
            P.I('dve', 'tensor_scalar', out=scale1m[:, s, :], in0=scale1[:], scalar1=maskc[:, s:s + 1],
                scalar2=None, op0=ALU.mult)
            P.I('dve', 'tensor_scalar', out=bias1m[:, s, :], in0=modc[:, 0:8], scalar1=maskc[:, s:s + 1],
                scalar2=None, op0=ALU.mult)
        P.I('dve', 'tensor_scalar', out=rtmp[:], in0=rowbuf[0:1, 2 * D:3 * D], scalar1=1.0, scalar2=None, op0=ALU.add)
        P.I('dve', 'tensor_tensor', out=rowbuf[0:1, 2 * D:3 * D], in0=rtmp[:], in1=nfwr[:], op=ALU.mult)
        bct = sb(sa, [128, D], name="bct")
        for ri in range(4):
            dst = gt1_b if ri == 0 else bct
            for hf in range(2):
                o = ri * D + hf * 512
                P.I('pe', 'matmul', out=bc_ps[:, 0:512], lhsT=ones_row[0:1, 0:128], rhs=rowbuf[0:1, o:o + 512],
                    start=True, stop=True)
                P.I('act', 'activation', out=dst[:, hf * 512:(hf + 1) * 512], in_=bc_ps[:, 0:512], func=AF.Identity)
            if ri > 0:
                P.dma('sp', out=bcs[ri - 1, :, :], in_=bct[:])
    P.barrier()

    if "B" in phases and lim >= 2:
      with ExitStack() as sB:
        win_bf = sb(sB, [128, KC, 2560], BF16, name="win_bf")
        wout_bf = sb(sB, [128, KC, D], BF16, name="wout_bf")
        glu_bf = sb(sB, [128, 4, 512], BF16, name="glu_bf")
        lb_b = sb(sB, [128, 512], name="lb_b")
        oml_b = sb(sB, [128, 512], name="oml_b")
        gnw_b = sb(sB, [128, 128], name="gnw_b")
        s5d_c = sb(sB, [128, 4], name="s5d_c")
        glub_c = sb(sB, [128, 4], name="glub_c")
        cosT = sb(sB, [128, 16, TC], name="cosT")
        sinT = sb(sB, [128, 16, TC], name="sinT")
        mag_p = sb(sB, [128, 16], name="mag_p")
        cs_p = sb(sB, [128, 16], name="cs_p")
        sn_p = sb(sB, [128, 16], name="sn_p")
        A256r = sb(sB, [128, 16], name="A256r")
        A256i = sb(sB, [128, 16], name="A256i")
        BbT = sb(sB, [128, 16, 256], BF16, name="BbT")
        Cb = sb(sB, [128, 16, 256], BF16, name="Cb")
        car_re = sb(sB, [128, 16], name="car_re")
        car_im = sb(sB, [128, 16], name="car_im")
        Sst = [sb(sB, [128, 128], name="S%d" % h) for h in range(4)]
        Sbf = [sb(sB, [128, 128], BF16, name="Sbf%d" % h) for h in range(4)]
        P.dma('sp', out=gnw_b[:], in_=gnw_d[:, :])
        P.dma('sp', out=s5d_c[:], in_=s5d_d[:, :])
        P.dma('sp', out=glub_c[:], in_=glub_d[:, :])
        pT0 = ps(sB, [128, 512], name="pT0")
        pT1 = ps(sB, [128, 512], name="pT1")
        pP = [ps(sB, [128, 512], name="pP%d" % i) for i in range(4)]
        pM = ps(sB, [128, 512], name="pM")
        pX = ps(sB, [128, 512], name="pX")
        pXb = pX[:].bitcast(BF16)

        with ExitStack() as s1:
            stg = [sb(s1, [128, 2560], name="stg%d" % i) for i in range(2)]
            li = [0]

            def load_cast(dst_fn, src_ap, rows_k, ncols):
                for k in range(rows_k):
                    st = stg[li[0] % 2]
                    P.dma('sp' if li[0] % 2 == 0 else 'act', out=st[:, 0:ncols], in_=src_ap[k * 128:(k + 1) * 128, :])
                    if li[0] % 2 == 0:
                        P.I('dve', 'tensor_copy', out=dst_fn(k), in_=st[:, 0:ncols])
                    else:
                        P.I('act', 'activation', out=dst_fn(k), in_=st[:, 0:ncols], func=AF.Identity)
                    li[0] += 1

            load_cast(lambda k: win_bf[:, k, :], win_d, KC, 2560)
            load_cast(lambda k: wout_bf[:, k, :], wout_d, KC, D)
            load_cast(lambda k: glu_bf[:, k, :], glu_d, 4, 512)

            hlb = sb(s1, [128, 2, 512], name="hlb")
            P.dma('sp', out=hlb[:], in_=hlb_d[:, :, :])
            P.I('dve', 'tensor_tensor', out=oml_b[:], in0=hlb[:, 0, :], in1=hlb[:, 1, :], op=ALU.subtract)
            P.I('act', 'activation', out=lb_b[:], in_=oml_b[:], func=AF.Sigmoid)
            P.I('dve', 'tensor_scalar', out=oml_b[:], in0=lb_b[:], scalar1=-1.0, scalar2=1.0, op0=ALU.mult, op1=ALU.add)

            s5sc = sb(s1, [128, 16, 3], name="s5sc")
            bpadj = [sb(s1, [128, 256], name="bpadj%d" % i) for i in range(2)]
            cpadj = [sb(s1, [128, 256], name="cpadj%d" % i) for i in range(2)]
            P.dma('sp', out=s5sc[:], in_=s5sc_d[:, :, :])
            nm = [0]

            def t16(name=None):
                nm[0] += 1
                return sb(s1, [128, 16], name=name or ("q16_%d" % nm[0]))

            ar, ai = s5sc[:, :, 0], s5sc[:, :, 1]
            dt_, th, mag = t16(), t16(), t16()
            P.I('act', 'activation', out=dt_[:], in_=s5sc[:, :, 2], func=AF.Exp)
            P.I('dve', 'tensor_tensor', out=th[:], in0=ai, in1=dt_[:], op=ALU.mult)
            P.I('dve', 'tensor_tensor', out=mag[:], in0=ar, in1=dt_[:], op=ALU.mult)
            P.I('act', 'activation', out=mag[:], in_=mag[:], func=AF.Exp)

            def sin_of(theta, shift):
                kf, ki, ph, m = t16(), sb(s1, [128, 16], I32), t16(), t16()
                res = t16()
                P.I('dve', 'tensor_scalar', out=ph[:], in0=theta[:], scalar1=shift, scalar2=None, op0=ALU.add)
                P.I('dve', 'tensor_scalar', out=kf[:], in0=ph[:], scalar1=1.0 / TWO_PI, scalar2=None, op0=ALU.mult)
                P.I('dve', 'tensor_copy', out=ki[:], in_=kf[:])
                P.I('dve', 'tensor_copy', out=kf[:], in_=ki[:])
                P.I('dve', 'scalar_tensor_tensor', out=ph[:], in0=kf[:], scalar=-TWO_PI, in1=ph[:], op0=ALU.mult, op1=ALU.add)
                P.I('dve', 'tensor_single_scalar', out=m[:], in_=ph[:], scalar=math.pi, op=ALU.is_gt)
                P.I('dve', 'scalar_tensor_tensor', out=ph[:], in0=m[:], scalar=-TWO_PI, in1=ph[:], op0=ALU.mult, op1=ALU.add)
                P.I('dve', 'tensor_single_scalar', out=m[:], in_=ph[:], scalar=-math.pi, op=ALU.is_lt)
                P.I('dve', 'scalar_tensor_tensor', out=ph[:], in0=m[:], scalar=TWO_PI, in1=ph[:], op0=ALU.mult, op1=ALU.add)
                P.I('act', 'activation', out=res[:], in_=ph[:], func=AF.Sin)
                return res

            sn = sin_of(th, 0.0)
            cs = sin_of(th, math.pi / 2)
            abr, abi, den, nr, cre, cim, tq = t16(), t16(), t16(), t16(), t16(), t16(), t16()
            P.I('dve', 'tensor_tensor', out=abr[:], in0=mag[:], in1=cs[:], op=ALU.mult)
            P.I('dve', 'tensor_tensor', out=abi[:], in0=mag[:], in1=sn[:], op=ALU.mult)
            P.I('dve', 'tensor_tensor', out=den[:], in0=ar, in1=ar, op=ALU.mult)
            P.I('dve', 'tensor_tensor', out=tq[:], in0=ai, in1=ai, op=ALU.mult)
            P.I('dve', 'tensor_tensor', out=den[:], in0=den[:], in1=tq[:], op=ALU.add)
            P.I('dve', 'reciprocal', out=den[:], in_=den[:])
            P.I('dve', 'tensor_scalar', out=nr[:], in0=abr[:], scalar1=-1.0, scalar2=None, op0=ALU.add)
            P.I('dve', 'tensor_tensor', out=cre[:], in0=nr[:], in1=ar, op=ALU.mult)
            P.I('dve', 'tensor_tensor', out=tq[:], in0=abi[:], in1=ai, op=ALU.mult)
            P.I('dve', 'tensor_tensor', out=cre[:], in0=cre[:], in1=tq[:], op=ALU.add)
            P.I('dve', 'tensor_tensor', out=cre[:], in0=cre[:], in1=den[:], op=ALU.mult)
            P.I('dve', 'tensor_tensor', out=cim[:], in0=abi[:], in1=ar, op=ALU.mult)
            P.I('dve', 'tensor_tensor', out=tq[:], in0=nr[:], in1=ai, op=ALU.mult)
            P.I('dve', 'tensor_tensor', out=cim[:], in0=cim[:], in1=tq[:], op=ALU.subtract)
            P.I('dve', 'tensor_tensor', out=cim[:], in0=cim[:], in1=den[:], op=ALU.mult)
            bb = sb(s1, [128, 256], name="bb")
            tb = sb(s1, [128, 128], name="tb")
            for j in range(16):
                bp, cp = bpadj[j % 2], cpadj[j % 2]
                P.dma('sp', out=bp[:], in_=bpad_d[j, :, :])
                P.dma('act', out=cp[:], in_=cpad_d[j, :, :])
                P.I('act', 'activation', out=Cb[:, j, 0:128], in_=cp[:, 0:128], func=AF.Identity)
                P.I('act', 'activation', out=Cb[:, j, 128:256], in_=cp[:, 128:256], func=AF.Identity, scale=-1.0)
                P.I('dve', 'tensor_scalar', out=tb[:], in0=bp[:, 128:256], scalar1=cim[:, j:j + 1], scalar2=None, op0=ALU.mult)
                P.I('dve', 'scalar_tensor_tensor', out=bb[:, 0:128], in0=bp[:, 0:128], scalar=cre[:, j:j + 1],
                    in1=tb[:], op0=ALU.mult, op1=ALU.subtract)
                P.I('dve', 'tensor_scalar', out=tb[:], in0=bp[:, 128:256], scalar1=cre[:, j:j + 1], scalar2=None, op0=ALU.mult)
                P.I('dve', 'scalar_tensor_tensor', out=bb[:, 128:256], in0=bp[:, 0:128], scalar=cim[:, j:j + 1],
                    in1=tb[:], op0=ALU.mult, op1=ALU.add)
                P.I('pe', 'transpose', out=pT0[:, 0:128], in_=bb[:, 0:128], identity=ident_f[:])
                P.I('pe', 'transpose', out=pT0[:, 128:256], in_=bb[:, 128:256], identity=ident_f[:])
                P.I('act', 'activation', out=BbT[:, j, :], in_=pT0[:, 0:256], func=AF.Identity)
            P.I('dve', 'tensor_copy', out=cs_p[:], in_=cs[:])
            P.I('dve', 'tensor_copy', out=sn_p[:], in_=sn[:])
            P.I('dve', 'memset', ap=cosT[:, :, TC - 1:TC], constant=1.0)
            P.I('dve', 'memset', ap=sinT[:, :, TC - 1:TC], constant=0.0)
            d1 = sb(s1, [128, 16, TC // 2], name="dbl1")
            d2 = sb(s1, [128, 16, TC // 2], name="dbl2")
            sre, sim, tq1 = t16(), t16(), t16()

            def cmul_scalar(lo):
                P.I('dve', 'tensor_tensor', out=sre[:], in0=cosT[:, :, lo], in1=abr[:], op=ALU.mult)
                P.I('dve', 'tensor_tensor', out=tq1[:], in0=sinT[:, :, lo], in1=abi[:], op=ALU.mult)
                P.I('dve', 'tensor_tensor', out=sre[:], in0=sre[:], in1=tq1[:], op=ALU.subtract)
                P.I('dve', 'tensor_tensor', out=sim[:], in0=cosT[:, :, lo], in1=abi[:], op=ALU.mult)
                P.I('dve', 'tensor_tensor', out=tq1[:], in0=sinT[:, :, lo], in1=abr[:], op=ALU.mult)
                P.I('dve', 'tensor_tensor', out=sim[:], in0=sim[:], in1=tq1[:], op=ALU.add)
            k = 1
            while k < TC:
                lo = TC - k
                cmul_scalar(lo)
                srb = sre[:].unsqueeze(2).to_broadcast([128, 16, k])
                sib = sim[:].unsqueeze(2).to_broadcast([128, 16, k])
                P.I('dve', 'tensor_tensor', out=d1[:, :, 0:k], in0=cosT[:, :, lo:TC], in1=srb, op=ALU.mult)
                P.I('dve', 'tensor_tensor', out=d2[:, :, 0:k], in0=sinT[:, :, lo:TC], in1=sib, op=ALU.mult)
                P.I('dve', 'tensor_tensor', out=cosT[:, :, lo - k:lo], in0=d1[:, :, 0:k], in1=d2[:, :, 0:k], op=ALU.subtract)
                P.I('dve', 'tensor_tensor', out=d1[:, :, 0:k], in0=cosT[:, :, lo:TC], in1=sib, op=ALU.mult)
                P.I('dve', 'tensor_tensor', out=d2[:, :, 0:k], in0=sinT[:, :, lo:TC], in1=srb, op=ALU.mult)
                P.I('dve', 'tensor_tensor', out=sinT[:, :, lo - k:lo], in0=d1[:, :, 0:k], in1=d2[:, :, 0:k], op=ALU.add)
                k *= 2
            cmul_scalar(0)
            P.I('dve', 'tensor_copy', out=A256r[:], in_=sre[:])
            P.I('dve', 'tensor_copy', out=A256i[:], in_=sim[:])
            P.I('dve', 'tensor_copy', out=mag_p[:], in_=mag[:])
            P.I('dve', 'memset', ap=car_re[:], constant=0.0)
            P.I('dve', 'memset', ap=car_im[:], constant=0.0)
            for h in range(4):
                P.I('dve', 'memset', ap=Sst[h][:], constant=0.0)
                P.I('dve', 'memset', ap=Sbf[h][:], constant=0.0)
        P.barrier()

        with ExitStack() as s2:
            xt = [sb(s2, [128, D], name="xt%d" % i) for i in range(2)]
            xn = sb(s2, [128, D], name="xn")
            ss = sb(s2, [128, 1], name="ss")
            hTs = [sb(s2, [128, KC, 512], BF16, name="hT%d" % i) for i in range(2)]
            uTs = [sb(s2, [128, 4, 512], BF16, name="uT%d" % i) for i in range(2)]
            du = sb(s2, [128, 4, 512], name="du")
            fsig = sb(s2, [128, 512], name="fsig")
            fv = fsig
            logf = sb(s2, [128, 512], name="logf")
            omf = sb(s2, [128, 512], name="omf")
            v_tm = sb(s2, [128, 512], BF16, name="v_tm")
            qs = sb(s2, [128, 512], name="qs")
            sg = sb(s2, [128, 512], name="sg")
            ep = sb(s2, [128, 128], name="ep")
            em = sb(s2, [128, 128], name="em")
            el = sb(s2, [128, 128], name="el")
            eb = sb(s2, [128, 2], name="eb")
            qd = sb(s2, [128, 128], BF16, name="qd")
            kd = sb(s2, [128, 128], BF16, name="kd")
            kdl = sb(s2, [128, 128], BF16, name="kdl")
            qkT = sb(s2, [128, 256], BF16, name="qkT")
            scm = sb(s2, [128, 128], BF16, name="scm")
            osq = sb(s2, [128, 1], name="osq")
            ojunk = sb(s2, [128, 128], name="ojunk")
            on = sb(s2, [128, 128], name="on")
            ob = sb(s2, [128, 128], BF16, name="ob")
            ohT = sb(s2, [128, 4, 512], BF16, name="ohT")
            s5t = [sb(s2, [128, TC], name="s5t%d" % i) for i in range(4)]
            wre = sb(s2, [128, TC], name="wre")
            wim = sb(s2, [128, TC], name="wim")
            rho_j = sb(s2, [128, TC], name="rho_j")
            tc1 = sb(s2, [128, 1], name="tc1")
            tc2 = sb(s2, [128, 1], name="tc2")
            xre = sb(s2, [128, 512], BF16, name="xre")
            xim = sb(s2, [128, 512], BF16, name="xim")
            yv = sb(s2, [128, 512], name="yv")
            yg = sb(s2, [128, 4, 512], BF16, name="yg")
            os5T = sb(s2, [128, 4, 512], BF16, name="os5T")
            acc = sb(s2, [128, 16, 8], name="acc")
            px = [s5t[2], s5t[3]]
            zre = sb(s2, [128, TC], name="zre")
            zim = sb(s2, [128, TC], name="zim")
            cq = [sb(s2, [128, 16], name="cq%d" % i) for i in range(4)]
            x1tmp = yv

            def is_own(blk):
                return blk // BPS == 3

            def emit_B1_tile(nb, tt):
                slot = nb // BPS
                hT = hTs[nb % 2]
                x_ = xt[tt % 2]
                pos = nb * 512 + tt * 128
                P.dma('sp', out=x_[:], in_=xin[pos:pos + 128, :])
                P.I('act', 'activation', out=xn[:], in_=x_[:], func=AF.Square, accum_out=ss[:])
                P.I('dve', 'tensor_scalar', out=ss[:], in0=ss[:], scalar1=1.0 / D, scalar2=EPS, op0=ALU.mult, op1=ALU.add)
                P.I('act', 'activation', out=ss[:], in_=ss[:], func=AF.Ln)
                P.I('act', 'activation', out=ss[:], in_=ss[:], func=AF.Exp, scale=-0.5)
                P.I('dve', 'tensor_scalar', out=xn[:], in0=x_[:], scalar1=ss[:, 0:1], scalar2=None, op0=ALU.mult)
                for k in range(KC):
                    pt = pT1 if k < 4 else pX
                    P.I('pe', 'transpose', out=pt[:, (k % 4) * 128:(k % 4 + 1) * 128], in_=xn[:, k * 128:(k + 1) * 128],
                        identity=ident_f[:])
                for k in range(KC):
                    pt = pT1 if k < 4 else pX
                    src = pt[:, (k % 4) * 128:(k % 4 + 1) * 128]
                    dst = hT[:, k, tt * 128:(tt + 1) * 128]
                    if k < 4:
                        P.I('act', 'activation', out=dst, in_=src, func=AF.Identity,
                            scale=scale1m[:, slot, k:k + 1], bias=bias1m[:, slot, k:k + 1])
                    else:
                        P.I('dve', 'tensor_scalar', out=dst, in0=src, scalar1=scale1m[:, slot, k:k + 1],
                            scalar2=bias1m[:, slot, k:k + 1], op0=ALU.mult, op1=ALU.add)

            def emit_B2_cg(nb, cg):
                hT, uT = hTs[nb % 2], uTs[nb % 2]
                pt = pT1 if cg % 2 == 0 else pX
                for k in range(KC):
                    P.I('pe', 'matmul', out=pt[:, 0:512], lhsT=win_bf[:, k, 2048 + cg * 128:2048 + (cg + 1) * 128],
                        rhs=hT[:, k, :], start=(k == 0), stop=(k == KC - 1))
                P.I('act', 'activation', out=uT[:, cg, :], in_=pt[:, 0:512], func=AF.Identity)
                if is_own(nb):
                    P.I('dve', 'tensor_scalar', out=du[:, cg, :], in0=pt[:, 0:512], scalar1=s5d_c[:, cg:cg + 1],
                        scalar2=None, op0=ALU.mult)

            def hg_tile(blk, tt, own):
                hT = hTs[blk % 2]
                sl = slice(tt * 128, (tt + 1) * 128)
                fams = [(0, 512), (1, 1024)] + ([(2, 0), (3, 1536)] if own else [])
                for (pi, c0) in fams:
                    for k in range(KC):
                        P.I('pe', 'matmul', out=pP[pi][:, 0:512], lhsT=hT[:, k, sl], rhs=win_bf[:, k, c0:c0 + 512],
                            start=(k == 0), stop=(k == KC - 1))
                P.I('act', 'activation', out=fsig[:], in_=pP[0][:, 0:512], func=AF.Sigmoid)
                P.I('dve', 'tensor_tensor', out=fv[:], in0=fsig[:], in1=oml_b[:], op=ALU.mult)
                P.I('dve', 'tensor_tensor', out=fv[:], in0=fv[:], in1=lb_b[:], op=ALU.add)
                P.I('act', 'activation', out=logf[:], in_=fv[:], func=AF.Ln)
                P.I('dve', 'tensor_scalar', out=omf[:], in0=fv[:], scalar1=-1.0, scalar2=1.0, op0=ALU.mult, op1=ALU.add)
                P.I('act', 'activation', out=v_tm[:], in_=pP[1][:, 0:512], func=AF.Identity)
                if own:
                    P.I('act', 'activation', out=qs[:], in_=pP[2][:, 0:512], func=AF.Silu)
                    P.I('act', 'activation', out=sg[:], in_=pP[3][:, 0:512], func=AF.Silu)

            def hg_unit(blk, tt, hd, own):
                sl = slice(tt * 128, (tt + 1) * 128)
                c = slice(hd * 128, (hd + 1) * 128)
                P.I('pe', 'matmul', out=pM[:, 0:128], lhsT=tri2[:], rhs=logf[:, c], start=True, stop=True)
                P.I('pe', 'matmul', out=pM[:, 128:256], lhsT=suf2[:], rhs=logf[:, c], start=True, stop=True)
                P.I('pe', 'matmul', out=pM[:, 384:386], lhsT=logf[:, c], rhs=chunkind[:], start=True, stop=True)
                if own:
                    P.I('act', 'activation', out=ep[:], in_=pM[:, 0:128], func=AF.Exp)
                    P.I('act', 'activation', out=em[:], in_=pM[:, 0:128], func=AF.Exp, scale=-1.0)
                P.I('act', 'activation', out=el[:], in_=pM[:, 128:256], func=AF.Exp)
                P.I('act', 'activation', out=eb[:], in_=pM[:, 384:386], func=AF.Exp)
                P.I('dve', 'tensor_tensor', out=kdl[:], in0=omf[:, c], in1=el[:], op=ALU.mult)
                if own:
                    P.I('dve', 'scalar_tensor_tensor', out=qd[:], in0=qs[:, c], scalar=DK ** -0.5, in1=ep[:],
                        op0=ALU.mult, op1=ALU.mult)
                    P.I('dve', 'tensor_tensor', out=kd[:], in0=omf[:, c], in1=em[:], op=ALU.mult)
                    P.I('pe', 'transpose', out=pXb[:, 0:128], in_=qd[:], identity=ident_b[:])
                    P.I('pe', 'transpose', out=pXb[:, 128:256], in_=kd[:], identity=ident_b[:])
                    P.I('act', 'activation', out=qkT[:], in_=pXb[:, 0:256], func=AF.Identity)
                    P.I('pe', 'matmul', out=pM[:, 256:384], lhsT=qkT[:, 128:256], rhs=qkT[:, 0:128], start=True, stop=True)
                    P.I('dve', 'tensor_tensor', out=scm[:], in0=pM[:, 256:384], in1=tri2[:], op=ALU.mult)
                    P.I('pe', 'matmul', out=pT1[:, 0:128], lhsT=scm[:], rhs=v_tm[:, c], start=True, stop=False)
                    P.I('pe', 'matmul', out=pT1[0:64, 0:128], lhsT=qkT[:, 0:64], rhs=Sbf[hd][:], start=False, stop=False)
                for c2 in range(2):
                    r = slice(c2 * 64, (c2 + 1) * 64)
                    P.I('pe', 'matmul', out=pT0[:, 0:128], lhsT=kdl[r, :], rhs=v_tm[r, c], start=True, stop=True)
                    P.I('dve', 'scalar_tensor_tensor', out=Sst[hd][:], in0=Sst[hd][:], scalar=eb[:, c2:c2 + 1],
                        in1=pT0[:, 0:128], op0=ALU.mult, op1=ALU.add)
                    P.I('act', 'activation', out=Sbf[hd][:], in_=Sst[hd][:], func=AF.Identity)
                    if own and c2 == 0:
                        P.I('pe', 'matmul', out=pT1[64:128, 0:128], lhsT=qkT[:, 64:128], rhs=Sbf[hd][:], start=False, stop=True)
                if own:
                    P.I('act', 'activation', out=ojunk[:], in_=pT1[:, 0:128], func=AF.Square, accum_out=osq[:])
                    P.I('dve', 'tensor_scalar', out=osq[:], in0=osq[:], scalar1=1.0 / DK, scalar2=EPS, op0=ALU.mult, op1=ALU.add)
                    P.I('act', 'activation', out=osq[:], in_=osq[:], func=AF.Ln)
                    P.I('act', 'activation', out=osq[:], in_=osq[:], func=AF.Exp, scale=-0.5)
                    P.I('dve', 'scalar_tensor_tensor', out=on[:], in0=pT1[:, 0:128], scalar=osq[:, 0:1], in1=gnw_b[:],
                        op0=ALU.mult, op1=ALU.mult)
                    P.I('dve', 'tensor_tensor', out=ob[:], in0=on[:], in1=sg[:, c], op=ALU.mult)
                    P.I('pe', 'transpose', out=pXb[:, 256:384], in_=ob[:], identity=ident_b[:])
                    P.I('act', 'activation', out=ohT[:, hd, sl], in_=pXb[:, 256:384], func=AF.Identity)

            def s5_prefix_j(blk, j):
                uT = uTs[blk % 2]
                cg = j // 4
                P.I('pe', 'matmul', out=pP[2][:, 0:512], lhsT=BbT[:, j, 0:128], rhs=uT[:, cg, :], start=True, stop=True)
                P.I('pe', 'matmul', out=pP[3][:, 0:512], lhsT=BbT[:, j, 128:256], rhs=uT[:, cg, :], start=True, stop=True)
                for hf in range(2):
                    cs_ = slice(hf * TC, (hf + 1) * TC)
                    for (ci, src, tab) in ((0, pP[2], cosT), (1, pP[3], sinT), (2, pP[3], cosT), (3, pP[2], sinT)):
                        P.I('dve', 'scalar_tensor_tensor', out=s5t[0][:], in0=src[:, cs_], scalar=1.0, in1=tab[:, j, :],
                            op0=ALU.mult, op1=ALU.mult, accum_out=acc[:, j, hf * 4 + ci:hf * 4 + ci + 1])

            def prefix_unit(blk, tt, hd, j):
                c = slice(hd * 128, (hd + 1) * 128)
                uT = uTs[blk % 2]
                cg = j // 4
                P.I('pe', 'matmul', out=pM[:, 128:256], lhsT=suf2[:], rhs=logf[:, c], start=True, stop=True)
                P.I('pe', 'matmul', out=pM[:, 384:386], lhsT=logf[:, c], rhs=chunkind[:], start=True, stop=True)
                P.I('pe', 'matmul', out=pP[2][:, 0:512], lhsT=BbT[:, j, 0:128], rhs=uT[:, cg, :], start=True, stop=True)
                P.I('pe', 'matmul', out=pP[3][:, 0:512], lhsT=BbT[:, j, 128:256], rhs=uT[:, cg, :], start=True, stop=True)
                P.I('act', 'activation', out=el[:], in_=pM[:, 128:256], func=AF.Exp)
                P.I('act', 'activation', out=eb[:], in_=pM[:, 384:386], func=AF.Exp)

                def stt(hf, ci):
                    src, tab = ((pP[2], cosT), (pP[3], sinT), (pP[3], cosT), (pP[2], sinT))[ci]
                    P.I('dve', 'scalar_tensor_tensor', out=s5t[0][:], in0=src[:, hf * TC:(hf + 1) * TC], scalar=1.0, in1=tab[:, j, :],
                        op0=ALU.mult, op1=ALU.mult, accum_out=acc[:, j, hf * 4 + ci:hf * 4 + ci + 1])
                stt(0, 0)
                stt(0, 1)
                P.I('dve', 'tensor_tensor', out=kdl[:], in0=omf[:, c], in1=el[:], op=ALU.mult)
                dsb = (pT0, pP[1])
                for c2 in range(2):
                    r = slice(c2 * 64, (c2 + 1) * 64)
                    P.I('pe', 'matmul', out=dsb[c2][:, 0:128], lhsT=kdl[r, :], rhs=v_tm[r, c], start=True, stop=True)
                stt(0, 2)
                stt(0, 3)
                for c2 in range(2):
                    P.I('dve', 'scalar_tensor_tensor', out=Sst[hd][:], in0=Sst[hd][:], scalar=eb[:, c2:c2 + 1],
                        in1=dsb[c2][:, 0:128], op0=ALU.mult, op1=ALU.add)
                for ci in range(4):
                    stt(1, ci)

            def s5_prefix_combine():
                for hf in range(2):
                    o = hf * 4
                    P.I('dve', 'tensor_tensor', out=cq[0][:], in0=A256r[:], in1=car_re[:], op=ALU.mult)
                    P.I('dve', 'tensor_tensor', out=cq[1][:], in0=A256i[:], in1=car_im[:], op=ALU.mult)
                    P.I('dve', 'tensor_tensor', out=cq[2][:], in0=A256r[:], in1=car_im[:], op=ALU.mult)
                    P.I('dve', 'tensor_tensor', out=cq[3][:], in0=A256i[:], in1=car_re[:], op=ALU.mult)
                    P.I('dve', 'tensor_tensor', out=car_re[:], in0=cq[0][:], in1=cq[1][:], op=ALU.subtract)
                    P.I('dve', 'tensor_tensor', out=car_im[:], in0=cq[2][:], in1=cq[3][:], op=ALU.add)
                    P.I('dve', 'tensor_tensor', out=car_re[:], in0=car_re[:], in1=acc[:, :, o + 0], op=ALU.add)
                    P.I('dve', 'tensor_tensor', out=car_re[:], in0=car_re[:], in1=acc[:, :, o + 1], op=ALU.subtract)
                    P.I('dve', 'tensor_tensor', out=car_im[:], in0=car_im[:], in1=acc[:, :, o + 2], op=ALU.add)
                    P.I('dve', 'tensor_tensor', out=car_im[:], in0=car_im[:], in1=acc[:, :, o + 3], op=ALU.add)

            def build_rot():
                P.I('dve', 'tensor_copy', out=cosT[:, :, 0], in_=cs_p[:])
                P.I('dve', 'tensor_copy', out=sinT[:, :, 0], in_=sn_p[:])
                d1 = xn[:].rearrange("p (j c) -> p j c", c=128)
                d2 = xt[0][:].rearrange("p (j c) -> p j c", c=128)
                for jh in range(2):
                    js = slice(jh * 8, (jh + 1) * 8)
                    k = 1
                    while k < TC:
                        cb_ = cosT[:, js, k - 1:k].to_broadcast([128, 8, k])
                        sb_ = sinT[:, js, k - 1:k].to_broadcast([128, 8, k])
                        P.I('dve', 'tensor_tensor', out=d1[:, :, 0:k], in0=cosT[:, js, 0:k], in1=cb_, op=ALU.mult)
                        P.I('dve', 'tensor_tensor', out=d2[:, :, 0:k], in0=sinT[:, js, 0:k], in1=sb_, op=ALU.mult)
                        P.I('dve', 'tensor_tensor', out=cosT[:, js, k:2 * k], in0=d1[:, :, 0:k], in1=d2[:, :, 0:k], op=ALU.subtract)
                        P.I('dve', 'tensor_tensor', out=d1[:, :, 0:k], in0=cosT[:, js, 0:k], in1=sb_, op=ALU.mult)
                        P.I('dve', 'tensor_tensor', out=d2[:, :, 0:k], in0=sinT[:, js, 0:k], in1=cb_, op=ALU.mult)
                        P.I('dve', 'tensor_tensor', out=sinT[:, js, k:2 * k], in0=d1[:, :, 0:k], in1=d2[:, :, 0:k], op=ALU.add)
                        k *= 2

            def s5_own_j(blk, j):
                uT = uTs[blk % 2]
                cg = j // 4
                P.I('pe', 'matmul', out=pM[:, 0:512], lhsT=BbT[:, j, 0:128], rhs=uT[:, cg, :], start=True, stop=True)
                P.I('pe', 'matmul', out=pT0[:, 0:512], lhsT=BbT[:, j, 128:256], rhs=uT[:, cg, :], start=True, stop=True)
                cosj, sinj = cosT[:, j, :], sinT[:, j, :]
                P.I('dve', 'tensor_copy', out=rho_j[:], in_=mag_p[:, j:j + 1].to_broadcast([128, TC]))
                for hf in range(2):
                    cs_ = slice(hf * TC, (hf + 1) * TC)
                    P.I('dve', 'tensor_tensor', out=s5t[0][:], in0=pM[:, cs_], in1=cosj, op=ALU.mult)
                    P.I('dve', 'tensor_tensor', out=s5t[1][:], in0=pT0[:, cs_], in1=sinj, op=ALU.mult)
                    P.I('dve', 'tensor_tensor', out=wre[:], in0=s5t[0][:], in1=s5t[1][:], op=ALU.add)
                    P.I('dve', 'tensor_tensor', out=s5t[0][:], in0=pT0[:, cs_], in1=cosj, op=ALU.mult)
                    P.I('dve', 'tensor_tensor', out=s5t[1][:], in0=pM[:, cs_], in1=sinj, op=ALU.mult)
                    P.I('dve', 'tensor_tensor', out=wim[:], in0=s5t[0][:], in1=s5t[1][:], op=ALU.subtract)
                    P.I('dve', 'tensor_tensor_scan', out=zre[:], data0=rho_j[:], data1=wre[:],
                        initial=car_re[:, j:j + 1], op0=ALU.mult, op1=ALU.add)
                    P.I('dve', 'tensor_tensor_scan', out=zim[:], data0=rho_j[:], data1=wim[:],
                        initial=car_im[:, j:j + 1], op0=ALU.mult, op1=ALU.add)
                    L = TC - 1
                    P.I('dve', 'tensor_scalar', out=tc1[:], in0=zim[:, L:L + 1], scalar1=sinT[:, j, L:L + 1], scalar2=None, op0=ALU.mult)
                    P.I('dve', 'tensor_scalar', out=tc2[:], in0=zim[:, L:L + 1], scalar1=cosT[:, j, L:L + 1], scalar2=None, op0=ALU.mult)
                    P.I('dve', 'scalar_tensor_tensor', out=car_re[:, j:j + 1], in0=zre[:, L:L + 1], scalar=cosT[:, j, L:L + 1],
                        in1=tc1[:], op0=ALU.mult, op1=ALU.subtract)
                    P.I('dve', 'scalar_tensor_tensor', out=car_im[:, j:j + 1], in0=zre[:, L:L + 1], scalar=sinT[:, j, L:L + 1],
                        in1=tc2[:], op0=ALU.mult, op1=ALU.add)
                    P.I('pool', 'tensor_tensor', out=px[0][:], in0=zre[:], in1=cosj, op=ALU.mult)
                    P.I('pool', 'tensor_tensor', out=px[1][:], in0=zim[:], in1=sinj, op=ALU.mult)
                    P.I('pool', 'tensor_tensor', out=xre[:, cs_], in0=px[0][:], in1=px[1][:], op=ALU.subtract)
                    P.I('pool', 'tensor_tensor', out=px[0][:], in0=zre[:], in1=sinj, op=ALU.mult)
                    P.I('pool', 'tensor_tensor', out=px[1][:], in0=zim[:], in1=cosj, op=ALU.mult)
                    P.I('pool', 'tensor_tensor', out=xim[:, cs_], in0=px[0][:], in1=px[1][:], op=ALU.add)
                P.I('pe', 'matmul', out=pP[cg][:, 0:512], lhsT=Cb[:, j, 0:128], rhs=xre[:], start=(j % 4 == 0), stop=False)
                P.I('pe', 'matmul', out=pP[cg][:, 0:512], lhsT=Cb[:, j, 128:256], rhs=xim[:], start=False, stop=(j % 4 == 3))

            def own_tail(blk):
                for cg in range(4):
                    P.I('dve', 'tensor_tensor', out=yv[:], in0=pP[cg][:, 0:512], in1=du[:, cg, :], op=ALU.add)
                    P.I('act', 'activation', out=yg[:, cg, :], in_=yv[:], func=AF.Gelu)
                for co in range(4):
                    for k in range(4):
                        P.I('pe', 'matmul', out=pP[co][:, 0:512], lhsT=glu_bf[:, k, co * 128:(co + 1) * 128], rhs=yg[:, k, :],
                            start=(k == 0), stop=(k == 3))
                    P.I('act', 'activation', out=yv[:], in_=pP[co][:, 0:512], func=AF.Sigmoid, bias=glub_c[:, co:co + 1])
                    P.I('dve', 'tensor_tensor', out=os5T[:, co, :], in0=yg[:, co, :], in1=yv[:], op=ALU.mult)
                xres = xt[0]
                for tt in range(4):
                    sl = slice(tt * 128, (tt + 1) * 128)
                    pos = blk * 512 + tt * 128
                    opos = pos - 3 * SEG
                    P.dma('sp', out=xres[:], in_=xin[pos:pos + 128, :])
                    for hf in range(2):
                        for k in range(KC):
                            lh = ohT[:, k, sl] if k < 4 else os5T[:, k - 4, sl]
                            P.I('pe', 'matmul', out=pP[hf][:, 0:512], lhsT=lh, rhs=wout_bf[:, k, hf * 512:(hf + 1) * 512],
                                start=(k == 0), stop=(k == KC - 1))
                        P.I('dve', 'tensor_tensor', out=x1tmp[:], in0=pP[hf][:, 0:512], in1=gt1_b[:, hf * 512:(hf + 1) * 512], op=ALU.mult)
                        P.I('dve', 'tensor_tensor', out=xres[:, hf * 512:(hf + 1) * 512], in0=x1tmp[:], in1=xres[:, hf * 512:(hf + 1) * 512], op=ALU.add)
                    P.dma('sp', out=x1s[opos:opos + 128, :], in_=xres[:])

            for tt in range(4):
                emit_B1_tile(0, tt)
            for cg in range(4):
                emit_B2_cg(0, cg)
            for blk in range(NBLK):
                own = is_own(blk)
                nb = blk + 1
                has_next = nb < NBLK
                if not own:
                    for i in range(16):
                        tt, hd = i // 4, i % 4
                        if has_next and hd == 0:
                            emit_B1_tile(nb, tt)
                        if hd == 0:
                            hg_tile(blk, tt, False)
                        prefix_unit(blk, tt, hd, i)
                        if has_next and i >= 13:
                            emit_B2_cg(nb, i - 13)
                    if has_next:
                        emit_B2_cg(nb, 3)
                    s5_prefix_combine()
                else:
                    if blk == 3 * BPS:
                        build_rot()
                        for hd in range(4):
                            P.I('act', 'activation', out=Sbf[hd][:], in_=Sst[hd][:], func=AF.Identity)
                    for tt in range(4):
                        hg_tile(blk, tt, True)
                        for hd in range(4):
                            hg_unit(blk, tt, hd, True)
                    for j in range(16):
                        s5_own_j(blk, j)
                    own_tail(blk)
                    if has_next:
                        for tt in range(4):
                            emit_B1_tile(nb, tt)
                        for cg in range(4):
                            emit_B2_cg(nb, cg)
        P.barrier()

    if "C" in phases:
      with ExitStack() as sC:
        wq_bf = sb(sC, [128, KC, 2048], BF16, name="wq_bf")
        keys_bf = sb(sC, [128, 256], BF16, name="keys_bf")
        gt2_b = sb(sC, [128, D], name="gt2_b")
        sh2_b = sb(sC, [128, D], name="sh2_b")
        sc2_b = sb(sC, [128, D], name="sc2_b")
        fnw_b = sb(sC, [128, D], name="fnw_bs")
        iota16 = sb(sC, [128, 16], name="iota16s")
        cA = [ps(sC, [128, 1024], name="cA%d" % i) for i in range(2)]
        cB = [ps(sC, [128, 1024], name="cB%d" % i) for i in range(2)]
        P.dma('sp', out=sh2_b[:], in_=bcs[0, :, :])
        P.dma('sp', out=sc2_b[:], in_=bcs[1, :, :])
        P.dma('sp', out=gt2_b[:], in_=bcs[2, :, :])
        P.dma('sp', out=fnw_b[:], in_=fnw_d[:, :])
        P.dma('sp', out=iota16[:], in_=iota16_d[:, :])
        with ExitStack() as s1:
            stg = [sb(s1, [128, 2048], name="stgc%d" % i) for i in range(2)]
            for k in range(KC):
                st = stg[k % 2]
                P.dma('sp' if k % 2 == 0 else 'act', out=st[:], in_=wq_d[k * 128:(k + 1) * 128, :])
                if k % 2 == 0:
                    P.I('dve', 'tensor_copy', out=wq_bf[:, k, :], in_=st[:])
                else:
                    P.I('act', 'activation', out=wq_bf[:, k, :], in_=st[:], func=AF.Identity)
            P.dma('sp', out=stg[0][:, 0:256], in_=keysT_d[:, :])
            P.I('dve', 'tensor_copy', out=keys_bf[:], in_=stg[0][:, 0:256])
        P.barrier()
        with ExitStack() as s2:
            x1ts = [sb(s2, [128, D], name="cx1t%d" % i) for i in range(2)]
            h2tms = [sb(s2, [128, D], BF16, name="ch2tm%d" % i) for i in range(2)]
            eidTs = [sb(s2, [128, 128], U32, name="ceidT%d" % i) for i in range(2)]
            gatesTs = [sb(s2, [128, 128], name="cgatesT%d" % i) for i in range(2)]
            xn2 = sb(s2, [128, D], name="cxn2")
            ss = sb(s2, [128, 1], name="css")
            ss2 = sb(s2, [128, 1], name="css2")
            h2f = sb(s2, [128, D], name="ch2f")
            x2f = sb(s2, [128, D], name="cx2f")
            h2T = sb(s2, [128, KC, 128], BF16, name="ch2T")
            qT = sb(s2, [128, 16, 128], BF16, name="cqT")
            sc = sb(s2, [128, 16, 128], name="csc")
            scw = sb(s2, [128, 16, 128], name="cscw")
            vals = sb(s2, [128, 8, 2, 16], name="cvals")
            idxu = sb(s2, [128, 8, 2, 16], U32, name="cidxu")
            idxf = sb(s2, [128, 8, 2, 16], name="cidxf")
            cand = sb(s2, [128, 8, 256], name="ccand")
            candw = sb(s2, [128, 8, 256], name="ccandw")
            best = sb(s2, [128, 8, 16], name="cbest")
            posu = sb(s2, [128, 8, 16], U32, name="cposu")
            piu = sb(s2, [128, 8, 16], U32, name="cpiu")
            pju = sb(s2, [128, 8, 16], U32, name="cpju")
            pif = sb(s2, [128, 8, 16], name="cpif")
            pjf = sb(s2, [128, 8, 16], name="cpjf")
            oh = sb(s2, [128, 8, 16, 16], name="coh")
            sel1 = sb(s2, [128, 8, 16], name="csel1")
            sel2 = sb(s2, [128, 8, 16], name="csel2")
            eidf = sb(s2, [128, 128], name="ceidf")
            gd = sb(s2, [128, 8, 16], name="cgd")
            gsum = sb(s2, [128, 8], name="cgsum")
            gates = sb(s2, [128, 128], name="cgates")
            NB = 8
            acol = [sb(s2, [128, 2], name="cacol%d" % i) for i in range(NB)]
            agcol = [sb(s2, [128, 2], name="cagcol%d" % i) for i in range(NB)]
            wcol = [sb(s2, [128, 2], BF16, name="cwcol%d" % i) for i in range(NB)]
            UV = [sb(s2, [128, 2 * D], BF16, name="cUV%d" % i) for i in range(NB)]
            junk = sb(s2, [128, D], BF16, name="cjunk")
            peerS = sb(s2, [128, D], name="cpeerS")
            ot = sb(s2, [128, D], name="cot")
            NEG = -1.0e30
            B4 = [128, 8, 16, 16]
            cR = cB[1]
            hbufs = [cA[0], cA[1]]

            def routing_ops(it):
                par = it % 2
                x1t, h2tm, eidT, gatesT = x1ts[par], h2tms[par], eidTs[par], gatesTs[par]
                tok0 = it * 128
                ops = []
                A = ops.append
                A(lambda: P.dma('sp', out=x1t[:], in_=x1s[tok0:tok0 + 128, :]))
                A(lambda: P.I('act', 'activation', out=xn2[:], in_=x1t[:], func=AF.Square, accum_out=ss[:]))
                A(lambda: P.I('dve', 'tensor_scalar', out=ss[:], in0=ss[:], scalar1=1.0 / D, scalar2=EPS, op0=ALU.mult, op1=ALU.add))
                A(lambda: P.I('act', 'activation', out=ss[:], in_=ss[:], func=AF.Ln))
                A(lambda: P.I('act', 'activation', out=ss[:], in_=ss[:], func=AF.Exp, scale=-0.5))
                A(lambda: P.I('dve', 'tensor_scalar', out=xn2[:], in0=x1t[:], scalar1=ss[:, 0:1], scalar2=None, op0=ALU.mult))
                A(lambda: P.I('dve', 'tensor_tensor', out=h2f[:], in0=xn2[:], in1=sc2_b[:], op=ALU.mult))
                A(lambda: P.I('dve', 'tensor_tensor', out=h2tm[:], in0=h2f[:], in1=sh2_b[:], op=ALU.add))
                for k in range(KC):
                    A(lambda k=k: P.I('pe', 'transpose', out=cR[:, k * 128:(k + 1) * 128], in_=xn2[:, k * 128:(k + 1) * 128], identity=ident_f[:]))
                for k in range(KC):
                    if k % 2 == 0:
                        A(lambda k=k: P.I('act', 'activation', out=h2T[:, k, :], in_=cR[:, k * 128:(k + 1) * 128], func=AF.Identity,
                                          scale=scale2c[:, k:k + 1], bias=sh2c[:, k:k + 1]))
                    else:
                        A(lambda k=k: P.I('dve', 'tensor_scalar', out=h2T[:, k, :], in0=cR[:, k * 128:(k + 1) * 128], scalar1=scale2c[:, k:k + 1],
                                          scalar2=sh2c[:, k:k + 1], op0=ALU.mult, op1=ALU.add))
                for half in range(2):
                    for b8 in range(8):
                        b16 = half * 8 + b8
                        for k in range(KC):
                            A(lambda b16=b16, b8=b8, k=k: P.I('pe', 'matmul', out=cR[:, b8 * 128:(b8 + 1) * 128],
                                                              lhsT=wq_bf[:, k, b16 * 128:(b16 + 1) * 128], rhs=h2T[:, k, :],
                                                              start=(k == 0), stop=(k == KC - 1)))
                    A(lambda half=half: P.I('act', 'activation', out=qT[:, half * 8:(half + 1) * 8, :],
                                            in_=cR[:].rearrange("p (b n) -> p b n", n=128), func=AF.Identity))
                for half in range(2):
                    for b8 in range(8):
                        b16 = half * 8 + b8
                        A(lambda b16=b16, b8=b8: P.I('pe', 'matmul', out=cR[:, b8 * 128:(b8 + 1) * 128], lhsT=qT[:, b16, :],
                                                     rhs=keys_bf[:, (b16 % 2) * 128:(b16 % 2 + 1) * 128], start=True, stop=True))
                    A(lambda half=half: P.I('act', 'activation', out=sc[:, half * 8:(half + 1) * 8, :],
                                            in_=cR[:].rearrange("p (b n) -> p b n", n=128), func=AF.Identity))
                for b16 in range(16):
                    hd, hf = b16 // 2, b16 % 2
                    A(lambda b16=b16, hd=hd, hf=hf: P.I('dve', 'max', out=vals[:, hd, hf, 0:8], in_=sc[:, b16, :]))
                    A(lambda b16=b16, hd=hd, hf=hf: P.I('dve', 'max_index', out=idxu[:, hd, hf, 0:8], in_max=vals[:, hd, hf, 0:8], in_values=sc[:, b16, :]))
                    A(lambda b16=b16, hd=hd, hf=hf: P.I('dve', 'match_replace', out=scw[:, b16, :], in_to_replace=vals[:, hd, hf, 0:8],
                                                        in_values=sc[:, b16, :], imm_value=NEG))
                    A(lambda b16=b16, hd=hd, hf=hf: P.I('dve', 'max', out=vals[:, hd, hf, 8:16], in_=scw[:, b16, :]))
                    A(lambda b16=b16, hd=hd, hf=hf: P.I('dve', 'max_index', out=idxu[:, hd, hf, 8:16], in_max=vals[:, hd, hf, 8:16], in_values=scw[:, b16, :]))
                A(lambda: P.I('dve', 'tensor_copy', out=idxf[:], in_=idxu[:]))
                A(lambda: P.I('dve', 'tensor_tensor', out=cand[:].rearrange("p h (i j) -> p h i j", j=16),
                              in0=vals[:, :, 0, :].unsqueeze(3).to_broadcast(B4), in1=vals[:, :, 1, :].unsqueeze(2).to_broadcast(B4), op=ALU.add))
                for hd in range(8):
                    A(lambda hd=hd: P.I('dve', 'max', out=best[:, hd, 0:8], in_=cand[:, hd, :]))
                    A(lambda hd=hd: P.I('dve', 'max_index', out=posu[:, hd, 0:8], in_max=best[:, hd, 0:8], in_values=cand[:, hd, :]))
                    A(lambda hd=hd: P.I('dve', 'match_replace', out=candw[:, hd, :], in_to_replace=best[:, hd, 0:8], in_values=cand[:, hd, :], imm_value=NEG))
                    A(lambda hd=hd: P.I('dve', 'max', out=best[:, hd, 8:16], in_=candw[:, hd, :]))
                    A(lambda hd=hd: P.I('dve', 'max_index', out=posu[:, hd, 8:16], in_max=best[:, hd, 8:16], in_values=candw[:, hd, :]))
                A(lambda: P.I('dve', 'tensor_single_scalar', out=piu[:], in_=posu[:], scalar=4, op=ALU.logical_shift_right))
                A(lambda: P.I('dve', 'tensor_single_scalar', out=pju[:], in_=posu[:], scalar=15, op=ALU.bitwise_and))
                A(lambda: P.I('dve', 'tensor_copy', out=pif[:], in_=piu[:]))
                A(lambda: P.I('dve', 'tensor_copy', out=pjf[:], in_=pju[:]))
                io_b = iota16[:].unsqueeze(1).unsqueeze(1).to_broadcast(B4)
                for (pf, hf, sel) in ((pif, 0, sel1), (pjf, 1, sel2)):
                    A(lambda pf=pf: P.I('dve', 'tensor_tensor', out=oh[:], in0=pf[:].unsqueeze(3).to_broadcast(B4), in1=io_b, op=ALU.is_equal))
                    A(lambda hf=hf: P.I('dve', 'tensor_tensor', out=oh[:], in0=oh[:], in1=idxf[:, :, hf, :].unsqueeze(2).to_broadcast(B4), op=ALU.mult))
                    A(lambda sel=sel: P.I('dve', 'tensor_reduce', out=sel[:], in_=oh[:], axis=AX.X, op=ALU.add))
                A(lambda: P.I('dve', 'scalar_tensor_tensor', out=eidf[:].rearrange("p (h k) -> p h k", k=16), in0=sel1[:], scalar=128.0, in1=sel2[:],
                              op0=ALU.mult, op1=ALU.add))
                A(lambda: P.I('dve', 'tensor_tensor', out=gd[:], in0=best[:], in1=best[:, :, 0:1].to_broadcast([128, 8, 16]), op=ALU.subtract))
                A(lambda: P.I('act', 'activation', out=gd[:], in_=gd[:], func=AF.Exp))
                A(lambda: P.I('dve', 'tensor_reduce', out=gsum[:], in_=gd[:], axis=AX.X, op=ALU.add))
                A(lambda: P.I('dve', 'reciprocal', out=gsum[:], in_=gsum[:]))
                A(lambda: P.I('dve', 'tensor_tensor', out=gates[:].rearrange("p (h k) -> p h k", k=16), in0=gd[:],
                              in1=gsum[:].unsqueeze(2).to_broadcast([128, 8, 16]), op=ALU.mult))
                A(lambda: P.I('pe', 'transpose', out=cR[:, 0:128], in_=eidf[:], identity=ident_f[:]))
                A(lambda: P.I('pe', 'transpose', out=cR[:, 128:256], in_=gates[:], identity=ident_f[:]))
                A(lambda: P.I('dve', 'tensor_copy', out=eidT[:], in_=cR[:, 0:128]))
                A(lambda: P.I('act', 'activation', out=gatesT[:], in_=cR[:, 128:256], func=AF.Identity))
                return ops

            def epilogue_ops(it):
                x1t = x1ts[it % 2]
                tok0 = it * 128
                ops = []
                A = ops.append
                A(lambda: P.I('act', 'activation', out=peerS[:], in_=cB[0][:], func=AF.Identity))
                for dk in range(KC):
                    A(lambda dk=dk: P.I('pe', 'transpose', out=cR[:, dk * 128:(dk + 1) * 128], in_=peerS[:, dk * 128:(dk + 1) * 128], identity=ident_f[:]))
                A(lambda: P.I('dve', 'tensor_tensor', out=x2f[:], in0=cR[:], in1=gt2_b[:], op=ALU.mult))
                A(lambda: P.I('dve', 'tensor_tensor', out=x2f[:], in0=x2f[:], in1=x1t[:], op=ALU.add))
                A(lambda: P.I('act', 'activation', out=ot[:], in_=x2f[:], func=AF.Square, accum_out=ss2[:]))
                A(lambda: P.I('dve', 'tensor_scalar', out=ss2[:], in0=ss2[:], scalar1=1.0 / D, scalar2=EPS, op0=ALU.mult, op1=ALU.add))
                A(lambda: P.I('act', 'activation', out=ss2[:], in_=ss2[:], func=AF.Ln))
                A(lambda: P.I('act', 'activation', out=ss2[:], in_=ss2[:], func=AF.Exp, scale=-0.5))
                A(lambda: P.I('dve', 'scalar_tensor_tensor', out=ot[:], in0=x2f[:], scalar=ss2[:, 0:1], in1=fnw_b[:], op0=ALU.mult, op1=ALU.mult))
                A(lambda: P.dma('sp', out=out_d[tok0:tok0 + 128, :], in_=ot[:]))
                return ops

            def token_loop(it, side_ops):
                eidT, gatesT, h2tm = eidTs[it % 2], gatesTs[it % 2], h2tms[it % 2]
                nside = len(side_ops)
                done = [0]

                def pump(upto):
                    while done[0] < min(upto, nside):
                        side_ops[done[0]]()
                        done[0] += 1

                def tail(t):
                    r = t % NB
                    P.I('dve', 'tensor_tensor', out=wcol[r][:, 0:1], in0=agcol[r][:, 0:1], in1=gatesT[:, t:t + 1], op=ALU.mult)
                    for dk in range(KC):
                        P.I('pe', 'matmul', out=cB[0][:, dk * 128 + t:dk * 128 + t + 1], lhsT=UV[r][:, D + dk * 128:D + (dk + 1) * 128],
                            rhs=wcol[r][:, 0:1], start=True, stop=True)
                pump(1)
                for t in range(128 + 2):
                    if t < 128:
                        r = t % NB
                        P.gather(UV[r][:], uvb[:, :], eidT[:, t:t + 1])
                        hb = hbufs[t % 2]
                        for hh in range(2):
                            P.I('pe', 'matmul', out=hb[:, hh * 512:(hh + 1) * 512], lhsT=ident_b[:, t:t + 1].to_broadcast([128, 128]),
                                rhs=h2tm[:, hh * 512:(hh + 1) * 512], start=True, stop=True)
                    if t >= 2:
                        tail(t - 2)
                    if t < 128:
                        P.I('dve', 'scalar_tensor_tensor', out=junk[:], in0=UV[r][:, 0:D], scalar=1.0, in1=hb[:], op0=ALU.mult, op1=ALU.mult,
                            accum_out=acol[r][:, 0:1])
                        P.I('act', 'activation', out=agcol[r][:, 0:1], in_=acol[r][:, 0:1], func=AF.Gelu)
                    pump(1 + ((t + 1) * nside) // 120)
                pump(nside)

            for op_ in routing_ops(0):
                op_()
            for it in range(NT):
                side = []
                if it > 0:
                    side += epilogue_ops(it - 1)
                if it + 1 < NT:
                    side += routing_ops(it + 1)
                token_loop(it, side)
            for op_ in epilogue_ops(NT - 1):
                op_()
        P.barrier()

    P.finish()
    print("instructions:", P.ninst)
    return nc


def prep(inputs, SEG):
    f = lambda a: np.ascontiguousarray(np.asarray(a, dtype=np.float32))
    x = f(inputs['x'])
    B, S, _ = x.shape
    assert S == 4 * SEG
    col8 = lambda v: f(v.reshape(8, 128).T)
    shared = {}
    shared['ada_w'] = f(inputs['ada_w'][0])
    ab = f(inputs['ada_b'][0])
    shared['ada_bc'] = f(ab.reshape(48, 128).T)
    shared['ada_br'] = f(ab.reshape(1, -1))
    shared['nmw_c'] = col8(f(inputs['norm_mix_w'][0]))
    shared['nfw_c'] = col8(f(inputs['norm_ffn_w'][0]))
    shared['nfw_r'] = f(inputs['norm_ffn_w'][0]).reshape(1, D)
    shared['fnw_b'] = f(np.broadcast_to(f(inputs['final_norm_w']).reshape(1, D), (128, D)))
    shared['w_in'] = f(inputs['w_in'][0])
    shared['w_out'] = f(inputs['w_out'][0])
    shared['wq'] = f(inputs['peer_wq'][0])
    shared['glu_w'] = f(inputs['s5_glu_w'][0])
    shared['hlb_b'] = f(np.broadcast_to(f(inputs['hg_lower_bounds']).reshape(1, 2, 512), (128, 2, 512)))
    shared['gnw_b'] = f(np.broadcast_to(f(inputs['hg_gnorm_w'][0]).reshape(1, 128), (128, 128)))
    a_re, a_im, ldt = f(inputs['s5_a_re'][0]), f(inputs['s5_a_im'][0]), f(inputs['s5_log_dt'][0])
    s5sc = np.zeros((128, 16, 3), np.float32)
    s5sc[:, :, 0] = a_re.reshape(16, 128).T
    s5sc[:, :, 1] = a_im.reshape(16, 128).T
    s5sc[:, :, 2] = np.repeat(ldt, 64).reshape(16, 128).T
    shared['s5sc'] = s5sc
    b_re, b_im = f(inputs['s5_b_re'][0]), f(inputs['s5_b_im'][0])
    c_re, c_im = f(inputs['s5_c_re'][0]), f(inputs['s5_c_im'][0])
    bpad = np.zeros((16, 128, 256), np.float32)
    cpad = np.zeros((16, 128, 256), np.float32)
    for j in range(16):
        for g2 in range(2):
            g = 2 * j + g2
            gl = 2 * (j % 4) + g2
            bpad[j, g2 * 64:(g2 + 1) * 64, gl * 16:(gl + 1) * 16] = b_re[g]
            bpad[j, g2 * 64:(g2 + 1) * 64, 128 + gl * 16:128 + (gl + 1) * 16] = b_im[g]
            cpad[j, g2 * 64:(g2 + 1) * 64, gl * 16:(gl + 1) * 16] = c_re[g].T
            cpad[j, g2 * 64:(g2 + 1) * 64, 128 + gl * 16:128 + (gl + 1) * 16] = c_im[g].T
    shared['bpad'] = bpad
    shared['cpad'] = cpad
    shared['s5d_c'] = f(f(inputs['s5_d'][0]).reshape(4, 128).T)
    shared['glub_c'] = f(f(inputs['s5_glu_b'][0]).reshape(4, 128).T)
    shared['keysT'] = f(np.concatenate([f(inputs['peer_keys1'][0]).T, f(inputs['peer_keys2'][0]).T], axis=1))
    shared['uv_tab'] = f(np.concatenate([f(inputs['peer_u'][0]), f(inputs['peer_v'][0])], axis=1))
    shared['ident'] = np.eye(128, dtype=np.float32)
    s_i = np.arange(128)
    same = (s_i[:, None] // 64) == (s_i[None, :] // 64)
    shared['tri2'] = (same & (s_i[:, None] <= s_i[None, :])).astype(np.float32)
    shared['suf2'] = (same & (s_i[:, None] > s_i[None, :])).astype(np.float32)
    ci = np.zeros((128, 2), np.float32)
    ci[:64, 0] = 1
    ci[64:, 1] = 1
    shared['chunkind'] = ci
    shared['iota16'] = f(np.broadcast_to(np.arange(16, dtype=np.float32).reshape(1, 16), (128, 16)))
    c = f(inputs['c'])
    in_maps = []
    for r in range(8):
        b, seg = r // 4, r % 4
        m = dict(shared)
        xin = np.zeros((4 * SEG, D), np.float32)
        npre = seg * SEG
        xin[3 * SEG - npre:3 * SEG] = x[b, 0:npre]
        xin[3 * SEG:] = x[b, seg * SEG:(seg + 1) * SEG]
        m['xin'] = xin
        mk = np.zeros((128, 4), np.float32)
        mk[:, 3 - seg:] = 1.0
        m['maskc'] = mk
        m['cT'] = col8(c[b])
        in_maps.append(m)
    return in_maps


_NC_CACHE = {}


def kernel(**inputs):
    S = np.asarray(inputs['x']).shape[1]
    SEG = S // 4
    if SEG not in _NC_CACHE:
        _NC_CACHE[SEG] = build(SEG)
    nc = _NC_CACHE[SEG]
    in_maps = prep(inputs, SEG)
    res = run_bass_kernel_spmd(nc, in_maps, core_ids=list(range(8)))
    out = np.zeros((2, S, D), np.float32)
    for r in range(8):
        b, seg = r // 4, r % 4
        out[b, seg * SEG:(seg + 1) * SEG] = res.results[r]["out"]
    return out
```
